# Optimizing a Trainium2 kernel written in Bass

```python
import math
import jax, jax.numpy as jnp
from jax import lax
import numpy as np

D_MODEL = 1024
BATCH = 8
SEQ = 2048
DEPTH = 2

CHUNK = 64
D_MIX = D_MODEL
GROUP_W = D_MIX // 4
EPS = 1e-6

SB_HEADS = 4
SB_HEAD_DIM = GROUP_W // SB_HEADS
SB_BLOCK = 128
RG_WIDTH = GROUP_W
RG_BLOCKS = 4
RG_BLOCK_DIM = RG_WIDTH // RG_BLOCKS
RG_CONV = 4
RG_C = 8.0
HG_HEADS = 4
HG_DK = GROUP_W // HG_HEADS
HG_DV = GROUP_W // HG_HEADS
M2_HEADS = 4
M2_HEAD_DIM = GROUP_W // M2_HEADS
M2_GROUPS = 2
M2_STATE = 128
M2_CONV = 4
M2_CONV_DIM = GROUP_W + 2 * M2_GROUPS * M2_STATE
N_EXPERTS = 32
TOP_K = 4
D_FF = D_MODEL
SWIGLU_LIMIT = 7.0
SWIGLU_ALPHA = 1.702
MOE_BLOCK = 128

IN_WIDTHS = ([GROUP_W] * 3
             + [GROUP_W] * 2
             + [GROUP_W] * 4
             + [GROUP_W, M2_CONV_DIM, M2_HEADS])
D_IN = sum(IN_WIDTHS)

kernel_name = "hybrid_sb_rglru_hgrn2_ssd_moe"


def rms_norm(x, g):
    xf = x.astype(jnp.float32)
    y = xf * lax.rsqrt(jnp.mean(xf * xf, axis=-1, keepdims=True) + EPS)
    return (y * g.astype(jnp.float32)).astype(x.dtype)


def group_rms(x, n_groups):
    shp = x.shape
    xg = x.reshape(shp[:-1] + (n_groups, shp[-1] // n_groups))
    xg = xg * lax.rsqrt(jnp.mean(xg * xg, axis=-1, keepdims=True) + EPS)
    return xg.reshape(shp)


def causal_depthwise_conv(x, w, b):
    k = w.shape[0]
    y = lax.conv_general_dilated(x, w[:, None, :], window_strides=(1,), padding=[(k - 1, 0)],
                                 dimension_numbers=("NWC", "WIO", "NWC"),
                                 feature_group_count=x.shape[-1])
    return y + b


def stick_breaking_attention(q, k, v):
    L = q.shape[2]
    scale = q.shape[-1] ** -0.5
    outs = []
    for blk in range(L // SB_BLOCK):
        q0 = blk * SB_BLOCK
        lk = q0 + SB_BLOCK
        z = jnp.einsum("bhtd,bhsd->bhts", q[:, :, q0:lk], k[:, :, :lk]).astype(jnp.float32) * scale
        t_idx = q0 + jnp.arange(SB_BLOCK)[:, None]
        s_idx = jnp.arange(lk)[None, :]
        mask = s_idx < t_idx
        log_keep = jnp.where(mask, jax.nn.log_sigmoid(-z), 0.0)
        log_remain = lax.cumsum(log_keep, axis=3, reverse=True) - log_keep
        log_a = jnp.where(mask, jax.nn.log_sigmoid(z) + log_remain, -jnp.inf)
        a = jnp.exp(log_a)
        outs.append(jnp.einsum("bhts,bhsd->bhtd", a.astype(v.dtype), v[:, :, :lk]))
    return jnp.concatenate(outs, axis=2)


def rg_lru_branch(x_in, gate_in, conv_w, conv_b, wa, ba, wx, bx, lam):
    B_, L, W = x_in.shape
    xc = causal_depthwise_conv(x_in, conv_w, conv_b)
    xb = xc.reshape(B_, L, RG_BLOCKS, RG_BLOCK_DIM)
    r = jax.nn.sigmoid((jnp.einsum("blni,nij->blnj", xb, wa).reshape(B_, L, W) + ba).astype(jnp.float32))
    ig = jax.nn.sigmoid((jnp.einsum("blni,nij->blnj", xb, wx).reshape(B_, L, W) + bx).astype(jnp.float32))
    log_a = -RG_C * r * jax.nn.softplus(-lam.astype(jnp.float32))
    a = jnp.exp(log_a)
    mult = jnp.sqrt(-jnp.expm1(2.0 * log_a))
    mult = jnp.where((jnp.arange(L) == 0)[None, :, None], 1.0, mult)
    u = mult * ig * xc.astype(jnp.float32)

    def combine(c1, c2):
        a1, b1 = c1
        a2, b2 = c2
        return a1 * a2, a2 * b1 + b2

    _, hs = lax.associative_scan(combine, (a, u), axis=1)
    return (hs * jax.nn.gelu(gate_in.astype(jnp.float32))).astype(x_in.dtype)


def hgrn2_branch(q_in, f_in, i_in, g_in, lb, norm_g):
    B_, L, W = q_in.shape
    nc = L // CHUNK
    forget = lb + (1.0 - lb) * jax.nn.sigmoid(f_in.astype(jnp.float32))
    log_f = jnp.log(forget)
    key = 1.0 - forget
    q = jax.nn.silu(q_in.astype(jnp.float32))
    v = i_in.astype(jnp.float32)

    def to_chunks(t, d):
        return t.reshape(B_, nc, CHUNK, HG_HEADS, d).transpose(1, 0, 3, 2, 4)

    qc, kc, lfc = to_chunks(q, HG_DK), to_chunks(key, HG_DK), to_chunks(log_f, HG_DK)
    vc = to_chunks(v, HG_DV)
    causal = jnp.tril(jnp.ones((CHUNK, CHUNK), bool))

    def step(state, inp):
        qb, kb, vb, lfb = inp
        bcum = jnp.cumsum(lfb, axis=2)
        diff = bcum[:, :, :, None, :] - bcum[:, :, None, :, :]
        decay = jnp.exp(jnp.where(causal[:, :, None], diff, -jnp.inf))
        scores = jnp.einsum("bhtc,bhsc,bhtsc->bhts", qb, kb, decay)
        o = jnp.einsum("bhts,bhsv->bhtv", scores, vb) + jnp.einsum("bhtc,bhcv->bhtv", qb * jnp.exp(bcum), state)
        b_last = bcum[:, :, -1:, :]
        new_state = (jnp.exp(b_last[:, :, 0, :, None]) * state
                     + jnp.einsum("bhsc,bhsv->bhcv", kb * jnp.exp(b_last - bcum), vb))
        return new_state, o

    s0 = jnp.zeros((B_, HG_HEADS, HG_DK, HG_DV), jnp.float32)
    _, o = lax.scan(step, s0, (qc, kc, vc, lfc))
    o = o.transpose(1, 0, 3, 2, 4).reshape(B_, L, W)
    o = group_rms(o, HG_HEADS) * norm_g.astype(jnp.float32) * jax.nn.silu(g_in.astype(jnp.float32))
    return o.astype(q_in.dtype)


def segsum(x):
    q = x.shape[-1]
    cs = jnp.cumsum(x, axis=-1)
    diff = cs[..., :, None] - cs[..., None, :]
    return jnp.where(jnp.tril(jnp.ones((q, q), bool)), diff, -jnp.inf)


def mamba2_branch(z, xbc, dt_raw, conv_w, conv_b, dt_bias, a_log, d_skip, norm_g):
    B_, L, _ = z.shape
    nc = L // CHUNK
    H, P, G, N = M2_HEADS, M2_HEAD_DIM, M2_GROUPS, M2_STATE
    xbc = jax.nn.silu(causal_depthwise_conv(xbc, conv_w, conv_b))
    xs = xbc[..., :GROUP_W].reshape(B_, L, H, P).astype(jnp.float32)
    bm = jnp.repeat(xbc[..., GROUP_W:GROUP_W + G * N].reshape(B_, L, G, N), H // G, axis=2).astype(jnp.float32)
    cm = jnp.repeat(xbc[..., GROUP_W + G * N:].reshape(B_, L, G, N), H // G, axis=2).astype(jnp.float32)
    dt = jax.nn.softplus(dt_raw.astype(jnp.float32) + dt_bias.astype(jnp.float32))
    da = dt * (-jnp.exp(a_log.astype(jnp.float32)))
    xd = xs * dt[..., None]
    xd_c = xd.reshape(B_, nc, CHUNK, H, P)
    b_c = bm.reshape(B_, nc, CHUNK, H, N)
    c_c = cm.reshape(B_, nc, CHUNK, H, N)
    da_c = da.reshape(B_, nc, CHUNK, H).transpose(0, 3, 1, 2)
    a_cs = jnp.cumsum(da_c, axis=-1)
    lmat = jnp.exp(segsum(da_c))
    y_diag = jnp.einsum("bclhn,bcshn,bhcls,bcshp->bclhp", c_c, b_c, lmat, xd_c)
    decay_states = jnp.exp(a_cs[..., -1:] - a_cs)
    states = jnp.einsum("bcshn,bhcs,bcshp->bchpn", b_c, decay_states, xd_c)
    chunk_decay = jnp.exp(a_cs[..., -1])

    def step(hstate, inp):
        st, dec = inp
        return dec[..., None, None] * hstate + st, hstate

    h0 = jnp.zeros((B_, H, P, N), jnp.float32)
    _, prev = lax.scan(step, h0, (states.transpose(1, 0, 2, 3, 4), chunk_decay.transpose(2, 0, 1)))
    prev = prev.transpose(1, 0, 2, 3, 4)
    y_off = jnp.einsum("bclhn,bchpn,bhcl->bclhp", c_c, prev, jnp.exp(a_cs))
    y = (y_diag + y_off).reshape(B_, L, H, P) + xs * d_skip.astype(jnp.float32)[:, None]
    y = y.reshape(B_, L, GROUP_W) * jax.nn.silu(z.astype(jnp.float32))
    y = group_rms(y, M2_GROUPS) * norm_g.astype(jnp.float32)
    return y.astype(z.dtype)


def moe_ffn(x, router_w, router_b, w_gu, b_gu, w_down, b_down):
    B_, L, D = x.shape
    n_tok = B_ * L
    xt = x.reshape(n_tok, D)
    logits = (xt @ router_w + router_b).astype(jnp.float32)
    top_logits, top_idx = lax.top_k(logits, TOP_K)
    gates = jax.nn.softmax(top_logits, axis=-1)
    n_assign = n_tok * TOP_K
    flat_e = top_idx.reshape(-1)
    order = jnp.argsort(flat_e)
    sorted_e = flat_e[order]
    sorted_tok = (order // TOP_K).astype(jnp.int32)
    counts = jnp.bincount(flat_e, length=N_EXPERTS)
    padded = (counts + MOE_BLOCK - 1) // MOE_BLOCK * MOE_BLOCK
    pad_end = jnp.cumsum(padded)
    pad_start = pad_end - padded
    start = jnp.cumsum(counts) - counts
    dest = (pad_start[sorted_e] + jnp.arange(n_assign) - start[sorted_e]).astype(jnp.int32)
    n_blocks = -(-n_assign // MOE_BLOCK) + N_EXPERTS
    slot_tok = jnp.zeros((n_blocks * MOE_BLOCK,), jnp.int32).at[dest].set(sorted_tok)
    block_expert = jnp.minimum(jnp.searchsorted(pad_end, jnp.arange(n_blocks) * MOE_BLOCK, side="right"),
                               N_EXPERTS - 1)

    def run_block(args):
        tok, e = args
        hmid = xt[tok] @ w_gu[e] + b_gu[e]
        glu = jnp.minimum(hmid[:, 0::2], SWIGLU_LIMIT)
        lin = jnp.clip(hmid[:, 1::2], -SWIGLU_LIMIT, SWIGLU_LIMIT)
        act = glu * jax.nn.sigmoid(SWIGLU_ALPHA * glu) * (lin + 1.0)
        return act @ w_down[e] + b_down[e]

    y_slots = lax.map(run_block, (slot_tok.reshape(n_blocks, MOE_BLOCK), block_expert)).reshape(-1, D)
    slot_of = jnp.zeros((n_assign,), jnp.int32).at[order].set(dest)
    y = jnp.einsum("tk,tkd->td", gates.astype(x.dtype), y_slots[slot_of].reshape(n_tok, TOP_K, D))
    return y.reshape(B_, L, D)


def setup_inputs(seed: int = 0) -> dict:
    key = jax.random.key(seed)
    ks = jax.random.split(key, 32)

    def nrm(k, shape, scale):
        return jax.random.normal(k, shape, jnp.float32) * scale

    def gain(k, shape):
        return 1.0 + 0.02 * jax.random.normal(k, shape, jnp.float32)

    x = nrm(ks[0], (BATCH, SEQ, D_MODEL), 1.0)
    norm_mix_g = gain(ks[1], (DEPTH, D_MODEL))
    w_in = nrm(ks[2], (DEPTH, D_MODEL, D_IN), D_MODEL ** -0.5)
    sb_norm_g = gain(ks[3], (DEPTH, GROUP_W))
    rg_conv_w = nrm(ks[4], (DEPTH, RG_CONV, RG_WIDTH), RG_CONV ** -0.5)
    rg_conv_b = nrm(ks[5], (DEPTH, RG_WIDTH), 0.01)
    rg_wa = nrm(ks[6], (DEPTH, RG_BLOCKS, RG_BLOCK_DIM, RG_BLOCK_DIM), RG_BLOCK_DIM ** -0.5)
    rg_ba = nrm(ks[7], (DEPTH, RG_WIDTH), 0.01)
    rg_wx = nrm(ks[8], (DEPTH, RG_BLOCKS, RG_BLOCK_DIM, RG_BLOCK_DIM), RG_BLOCK_DIM ** -0.5)
    rg_bx = nrm(ks[9], (DEPTH, RG_WIDTH), 0.01)
    a_pow = jax.random.uniform(ks[10], (DEPTH, RG_WIDTH), jnp.float32, 0.9, 0.999)
    s = a_pow ** (1.0 / RG_C)
    rg_lambda = jnp.log(s) - jnp.log1p(-s)
    rg_norm_g = gain(ks[11], (DEPTH, GROUP_W))
    hg_lower_bounds = nrm(ks[12], (DEPTH, GROUP_W), 0.1)
    hg_norm_g = gain(ks[13], (DEPTH, GROUP_W))
    m2_conv_w = nrm(ks[14], (DEPTH, M2_CONV, M2_CONV_DIM), M2_CONV ** -0.5)
    m2_conv_b = nrm(ks[15], (DEPTH, M2_CONV_DIM), 0.01)
    dt0 = jnp.exp(jax.random.uniform(ks[16], (DEPTH, M2_HEADS), jnp.float32, math.log(1e-3), math.log(1e-1)))
    m2_dt_bias = dt0 + jnp.log(-jnp.expm1(-dt0))
    m2_a_log = jnp.log(jax.random.uniform(ks[17], (DEPTH, M2_HEADS), jnp.float32, 1.0, 16.0))
    m2_d = gain(ks[18], (DEPTH, M2_HEADS))
    m2_norm_g = gain(ks[19], (DEPTH, GROUP_W))
    w_out = nrm(ks[20], (DEPTH, D_MIX, D_MODEL), 0.5 * D_MIX ** -0.5)
    norm_ffn_g = gain(ks[21], (DEPTH, D_MODEL))
    router_w = nrm(ks[22], (DEPTH, D_MODEL, N_EXPERTS), D_MODEL ** -0.5)
    router_b = nrm(ks[23], (DEPTH, N_EXPERTS), 0.01)
    moe_w_gu = nrm(ks[24], (DEPTH, N_EXPERTS, D_MODEL, 2 * D_FF), D_MODEL ** -0.5)
    moe_b_gu = nrm(ks[25], (DEPTH, N_EXPERTS, 2 * D_FF), 0.01)
    moe_w_down = nrm(ks[26], (DEPTH, N_EXPERTS, D_FF, D_MODEL), 0.5 * D_FF ** -0.5)
    moe_b_down = nrm(ks[27], (DEPTH, N_EXPERTS, D_MODEL), 0.01)
    final_norm_g = gain(ks[28], (D_MODEL,))
    return {"x": x, "norm_mix_g": norm_mix_g, "w_in": w_in, "sb_norm_g": sb_norm_g,
            "rg_conv_w": rg_conv_w, "rg_conv_b": rg_conv_b, "rg_wa": rg_wa, "rg_ba": rg_ba,
            "rg_wx": rg_wx, "rg_bx": rg_bx, "rg_lambda": rg_lambda, "rg_norm_g": rg_norm_g,
            "hg_lower_bounds": hg_lower_bounds, "hg_norm_g": hg_norm_g,
            "m2_conv_w": m2_conv_w, "m2_conv_b": m2_conv_b, "m2_dt_bias": m2_dt_bias,
            "m2_a_log": m2_a_log, "m2_d": m2_d, "m2_norm_g": m2_norm_g, "w_out": w_out,
            "norm_ffn_g": norm_ffn_g, "router_w": router_w, "router_b": router_b,
            "moe_w_gu": moe_w_gu, "moe_b_gu": moe_b_gu, "moe_w_down": moe_w_down,
            "moe_b_down": moe_b_down, "final_norm_g": final_norm_g}


def reference(x, norm_mix_g, w_in, sb_norm_g, rg_conv_w, rg_conv_b, rg_wa, rg_ba, rg_wx, rg_bx,
              rg_lambda, rg_norm_g, hg_lower_bounds, hg_norm_g, m2_conv_w, m2_conv_b, m2_dt_bias,
              m2_a_log, m2_d, m2_norm_g, w_out, norm_ffn_g, router_w, router_b, moe_w_gu, moe_b_gu,
              moe_w_down, moe_b_down, final_norm_g):
    B_, L, _ = x.shape
    lbs = jnp.cumsum(jax.nn.softmax(hg_lower_bounds.astype(jnp.float32), axis=0), axis=0)
    lbs = lbs - lbs[0]
    split_at = np.cumsum(IN_WIDTHS)[:-1].tolist()

    def to_heads(t, n):
        return t.reshape(B_, L, n, -1).transpose(0, 2, 1, 3)

    h = x
    for l in range(DEPTH):
        u = rms_norm(h, norm_mix_g[l])
        proj = u @ w_in[l]
        (sb_q, sb_k, sb_v, rg_x, rg_g, hg_q, hg_f, hg_i, hg_g,
         m2_z, m2_xbc, m2_dt) = jnp.split(proj, split_at, axis=-1)
        o_a = stick_breaking_attention(to_heads(sb_q, SB_HEADS), to_heads(sb_k, SB_HEADS), to_heads(sb_v, SB_HEADS))
        o_a = rms_norm(o_a.transpose(0, 2, 1, 3).reshape(B_, L, GROUP_W), sb_norm_g[l])
        o_b = rms_norm(rg_lru_branch(rg_x, rg_g, rg_conv_w[l], rg_conv_b[l], rg_wa[l], rg_ba[l],
                                     rg_wx[l], rg_bx[l], rg_lambda[l]), rg_norm_g[l])
        o_c = hgrn2_branch(hg_q, hg_f, hg_i, hg_g, lbs[l], hg_norm_g[l])
        o_d = mamba2_branch(m2_z, m2_xbc, m2_dt, m2_conv_w[l], m2_conv_b[l], m2_dt_bias[l],
                            m2_a_log[l], m2_d[l], m2_norm_g[l])
        mix = jnp.concatenate([o_a, o_b, o_c, o_d], axis=-1)
        h = h + mix @ w_out[l]
        h = h + moe_ffn(rms_norm(h, norm_ffn_g[l]), router_w[l], router_b[l], moe_w_gu[l],
                        moe_b_gu[l], moe_w_down[l], moe_b_down[l])
    return rms_norm(h, final_norm_g)
```

```python
import os
import types
import numpy as np
from contextlib import ExitStack
import concourse.bass as bass
import concourse.mybir as mybir
from concourse.bass_utils import run_bass_kernel_spmd

F32 = mybir.dt.float32
F32R = mybir.dt.float32r
BF16 = mybir.dt.bfloat16
I32 = mybir.dt.int32
U32 = mybir.dt.uint32
AF = mybir.ActivationFunctionType
ALU = mybir.AluOpType
AX = mybir.AxisListType

T = 2048
D = 1024
NT = 16
DEPTH = 2
DIN = 3332
NE = 32
CAP = 512
NCT = CAP // 128
EPS = 1e-6
NEG = -30000.0
NPP = 80

ENGS = ["pe", "act", "dve", "pool", "sp"]
N_DMA_SEMS = 40
ARENA_WORDS = 30208
ARENA_R_WORDS = 22528


def freeze(fn):
    if fn.__closure__ is None:
        return fn
    cells = []
    for c in fn.__closure__:
        try:
            cells.append(types.CellType(c.cell_contents))
        except ValueError:
            cells.append(c)
    return types.FunctionType(fn.__code__, fn.__globals__, fn.__name__, fn.__defaults__, tuple(cells))


class Prog:
    def __init__(self, nc):
        self.nc = nc
        self.es = ExitStack()
        self.ops = {e: [] for e in ENGS}
        self.cnt = {e: 0 for e in ENGS}
        self.pending = {e: False for e in ENGS}
        self.known = {e: {} for e in ENGS}
        self.last_w = {}
        self.readers = {}
        self.esem = {e: self.es.enter_context(nc.semaphore("sem_" + e)) for e in ENGS}
        self.dsem = [self.es.enter_context(nc.semaphore("dsem%d" % i)) for i in range(N_DMA_SEMS)]
        self.dval = [0] * N_DMA_SEMS
        self.isem = [self.es.enter_context(nc.semaphore("isem%d" % i)) for i in range(12)]
        self.iused = [False] * 12
        self.dnext = 0
        self.n_inst = 0
        self.arena = self.es.enter_context(nc.sbuf_tensor("arena", [128, ARENA_WORDS], F32))
        self.arena_r = self.es.enter_context(nc.sbuf_tensor("arena_r", [128, ARENA_R_WORDS], F32R))
        self.top_r = 0
        self.psum = self.es.enter_context(nc.psum_tensor("psum", [128, 8, 512], F32))
        self.top = 0
        self.uid = 0

    def alloc(self, words):
        off = self.top
        self.top += words
        assert self.top <= ARENA_WORDS, (self.top, ARENA_WORDS)
        return self.arena[:, off:off + words]

    def tile(self, shape, dt=F32):
        n = int(np.prod(shape))
        words = n if dt in (F32, F32R, I32, U32) else (n + 1) // 2
        if dt == F32R:
            off = self.top_r
            self.top_r += words
            assert self.top_r <= ARENA_R_WORDS, (self.top_r, ARENA_R_WORDS)
            ap = self.arena_r[:, off:off + words]
        else:
            ap = self.alloc(words)
            if dt != F32:
                ap = ap.bitcast(dt)
        if len(shape) == 2:
            ap = ap.rearrange("p (a b) -> p a b", b=shape[1])
        elif len(shape) == 3:
            ap = ap.rearrange("p (a b c) -> p a b c", b=shape[1], c=shape[2])
        return ap

    def name(self, base):
        self.uid += 1
        return "%s#%d" % (base, self.uid)

    def mark(self):
        return (self.top, self.top_r)

    def release(self, m):
        self.barrier()
        self.top, self.top_r = m

    def _need(self, eng, prod, waits):
        if prod is None:
            return
        if prod[0] == "e":
            _, e, idx = prod
            if e == eng and eng == "pe":
                return
            key = ("e", e)
        elif prod[0] == "i":
            waits[("i", prod[1], prod[2])] = 16
            return
        else:
            _, s, idx = prod
            key = ("d", s)
        if self.known[eng].get(key, 0) >= idx:
            return
        if idx > waits.get(key, 0):
            waits[key] = idx

    def _deps(self, eng, reads, writes):
        waits = {}
        for t in reads:
            self._need(eng, self.last_w.get(t), waits)
        for t in writes:
            self._need(eng, self.last_w.get(t), waits)
            for r in self.readers.get(t, ()):
                self._need(eng, r, waits)
        for key, idx in waits.items():
            if key[0] == "i":
                self.ops[eng].append(("wait", self.isem[key[1]], 16))
                continue
            self.known[eng][key] = idx
            sem = self.esem[key[1]] if key[0] == "e" else self.dsem[key[1]]
            self.ops[eng].append(("wait", sem, idx))

    def _mark(self, me, reads, writes):
        for t in reads:
            lst = self.readers.setdefault(t, [])
            lst[:] = [r for r in lst if not (r[0] == me[0] and r[1] == me[1])]
            lst.append(me)
        for t in writes:
            self.last_w[t] = me
            self.readers[t] = []

    def op(self, eng, fn, reads=(), writes=(), inc=True):
        fn = freeze(fn)
        self._deps(eng, reads, writes)
        if inc:
            self.cnt[eng] += 1
            idx = self.cnt[eng]
            self.pending[eng] = False
        else:
            idx = self.cnt[eng] + 1
            self.pending[eng] = True
        self.ops[eng].append(("inst", fn, inc))
        self._mark(("e", eng, idx), reads, writes)
        self.n_inst += 1

    def dma(self, q, fn, reads=(), writes=()):
        fn = freeze(fn)
        if q == "pool" and os.environ.get("SIMSEM"):
            sem = self.es.enter_context(self.nc.semaphore(self.name("fsem")))
            self.dsem.append(sem)
            self.dval.append(0)
            s = len(self.dsem) - 1
            self._deps(q, reads, writes)
            self.dval[s] = 16
            self.ops[q].append(("dma", fn, sem))
            self._mark(("d", s, 16), reads, writes)
            return
        s = self.dnext
        self.dnext = (self.dnext + 1) % N_DMA_SEMS
        if self.dval[s] > 0 and self.known[q].get(("d", s), 0) < self.dval[s]:
            self.known[q][("d", s)] = self.dval[s]
            self.ops[q].append(("wait", self.dsem[s], self.dval[s]))
        self._deps(q, reads, writes)
        self.dval[s] += 16
        self.ops[q].append(("dma", fn, self.dsem[s]))
        self._mark(("d", s, self.dval[s]), reads, writes)
        self.n_inst += 1

    def idma(self, slot, fn, reads=(), writes=(), wait_prev=False):
        q = "pool"
        if self.iused[slot]:
            if wait_prev:
                self.ops[q].append(("wait", self.isem[slot], 16))
            self.ops[q].append(("clear", self.isem[slot]))
        self._deps(q, reads, writes)
        self.iused[slot] = True
        self.uid += 1
        self.ops[q].append(("dma", fn, self.isem[slot]))
        self._mark(("i", slot, self.uid), reads, writes)
        self.n_inst += 1

    def isettle(self, slots, tokens, scratch):
        for sl in slots:
            if self.iused[sl]:
                self.ops["pool"].append(("wait", self.isem[sl], 16))
                self.ops["pool"].append(("clear", self.isem[sl]))
                self.iused[sl] = False
        self.cnt["pool"] += 1
        self.pending["pool"] = False
        self.ops["pool"].append(("inst", lambda e: e.memset(scratch, 0.0), True))
        me = ("e", "pool", self.cnt["pool"])
        for t in tokens:
            self.last_w[t] = me
            self.readers[t] = []

    def barrier(self):
        for f in ENGS:
            for e in ENGS:
                if e == f:
                    continue
                assert not self.pending[e]
                v = self.cnt[e]
                if v > self.known[f].get(("e", e), 0):
                    self.known[f][("e", e)] = v
                    self.ops[f].append(("wait", self.esem[e], v))
            for s in range(len(self.dsem)):
                v = self.dval[s]
                if v > self.known[f].get(("d", s), 0):
                    self.known[f][("d", s)] = v
                    self.ops[f].append(("wait", self.dsem[s], v))

    def finish(self, final_tokens):
        self._deps("sp", list(final_tokens), [])
        for e in ENGS:
            assert not self.pending[e], e
        nc = self.nc
        engmap = {"pe": "tensor", "act": "scalar", "dve": "vector", "pool": "gpsimd", "sp": "sync"}
        with nc.Block() as block:
            for e in ENGS:
                ops = self.ops[e]
                sem = self.esem[e]

                def body(engine, ops=ops, sem=sem):
                    for o in ops:
                        if o[0] == "wait":
                            engine.wait_ge(o[1], o[2])
                        elif o[0] == "clear":
                            engine.sem_clear(o[1])
                        elif o[0] == "inst":
                            ins = o[1](engine)
                            if o[2]:
                                ins.then_inc(sem, 1)
                        else:
                            o[1](engine).then_inc(o[2], 16)

                getattr(block, engmap[e])(body)
        self.es.close()

    def mm(self, out, lhsT, rhs, start, stop, reads, writes, inc=True):
        self.op("pe", lambda e: e.matmul(out, lhsT, rhs, start=start, stop=stop, skip_group_check=True), reads, writes, inc)

    def tr(self, out, in_, ident, reads, writes, inc=True):
        self.op("pe", lambda e: e.transpose(out, in_, ident), reads, writes, inc)

    def act(self, out, in_, func, reads, writes, bias=None, scale=None, accum_out=None):
        kw = {}
        if bias is not None:
            kw["bias"] = bias
        if scale is not None:
            kw["scale"] = scale
        if accum_out is not None:
            kw["accum_out"] = accum_out
        self.op("act", lambda e: e.activation(out, in_, func, **kw), reads, writes)

    def tt(self, eng, out, in0, in1, op, reads, writes):
        self.op(eng, lambda e: e.tensor_tensor(out, in0, in1, op), reads, writes)

    def ts(self, eng, out, in0, s1, s2, op0, op1, reads, writes):
        if s2 is None:
            self.op(eng, lambda e: e.tensor_scalar(out, in0, s1, None, op0), reads, writes)
        else:
            self.op(eng, lambda e: e.tensor_scalar(out, in0, s1, s2, op0, op1), reads, writes)

    def stt(self, eng, out, in0, scalar, in1, op0, op1, reads, writes):
        self.op(eng, lambda e: e.scalar_tensor_tensor(out, in0, scalar, in1, op0, op1), reads, writes)

    def cp(self, eng, out, in_, reads, writes):
        if eng == "act":
            self.op("act", lambda e: e.copy(out, in_), reads, writes)
        else:
            self.op(eng, lambda e: e.tensor_copy(out, in_), reads, writes)

    def memset(self, eng, out, val, writes):
        self.op(eng, lambda e: e.memset(out, val), (), writes)


def host_consts():
    c = {}
    c["ident"] = np.eye(128, dtype=np.float32)
    j = np.arange(128)[:, None]
    s = np.arange(128)[None, :]
    c["trim"] = (-1.0 * (j >= s)).astype(np.float32)
    c["ones"] = np.ones((128, 128), np.float32)
    c["bones"] = ((j // 64) == (s // 64)).astype(np.float32)
    c["maskle"] = (j <= s).astype(np.float32)
    c["negle"] = (NEG * (j > s)).astype(np.float32)
    e = np.zeros((128, 128), np.float32)
    e[127, :] = 1.0
    c["e127"] = e
    m64 = np.zeros((128, 2, 64), np.float32)
    ss_ = np.arange(128)[:, None]
    tt_ = np.arange(64)[None, :]
    m64[:, 0, :] = (ss_ < 64) & (ss_ <= tt_)
    m64[:, 1, :] = (ss_ >= 64) & (ss_ - 64 <= tt_)
    c["mask64"] = m64
    c["stri"] = (j < s).astype(np.float32)
    c["zeros"] = np.zeros((1024, D), np.float32)
    c["iota32"] = np.tile(np.arange(32, dtype=np.float32)[None, :], (128, 1))
    nm = np.zeros((128, 4, 512), np.float32)
    for jb in range(4):
        ss = jb * 128 + np.arange(128)[:, None]
        tt = np.arange(512)[None, :]
        nm[:, jb, :] = NEG * (ss >= tt)
    c["negmask"] = nm
    selh = np.zeros((36, 4, 128), np.float32)
    for h in range(4):
        selh[32 + h, h, :] = 1.0
    c["selh"] = selh
    selg = np.zeros((36, 2, 128), np.float32)
    for g in range(2):
        for hh in range(2):
            selg[32 + 2 * g + hh, g, hh * 64:(hh + 1) * 64] = 1.0
    c["selg"] = selg
    return c


def pk(v):
    return np.ascontiguousarray(v.reshape(-1, 128).T)


def host_prep(inp, moe=True):
    w = {}
    pp = np.zeros((DEPTH, 128, NPP), np.float32)
    for l in range(DEPTH):
        pp[l, :, 0:8] = pk(inp["norm_mix_g"][l])
        pp[l, :, 8:16] = pk(inp["norm_ffn_g"][l])
        pp[l, :, 16:18] = pk(inp["sb_norm_g"][l])
        pp[l, :, 18:20] = pk(inp["rg_norm_g"][l])
        pp[l, :, 20:22] = pk(inp["hg_norm_g"][l])
        pp[l, :, 22:24] = pk(inp["m2_norm_g"][l])
        for k in range(4):
            pp[l, :, 24 + k:24 + 8:4] = pk(inp["rg_conv_w"][l, k])
        pp[l, :, 32:34] = pk(inp["rg_conv_b"][l])
        pp[l, :, 34:36] = pk(inp["rg_ba"][l])
        pp[l, :, 36:38] = pk(inp["rg_bx"][l])
        pp[l, :, 38:40] = pk(inp["rg_lambda"][l])
        pp[l, :, 40:42] = pk(inp["hg_lower_bounds"][0])
        pp[l, :, 42:44] = pk(inp["hg_lower_bounds"][1])
        for k in range(4):
            pp[l, :, 44 + k:44 + 24:4] = pk(inp["m2_conv_w"][l, k])
        pp[l, :, 68:74] = pk(inp["m2_conv_b"][l])
        for g in range(2):
            pp[l, 0:64, 74 + g] = inp["m2_d"][l, 2 * g]
            pp[l, 64:128, 74 + g] = inp["m2_d"][l, 2 * g + 1]
        pp[l, 0:4, 76] = inp["m2_dt_bias"][l]
        pp[l, 32:36, 76] = inp["m2_dt_bias"][l]
        pp[l, 0:4, 77] = inp["m2_a_log"][l]
        pp[l, 32:36, 77] = inp["m2_a_log"][l]
    w["pp"] = pp
    w["w_in"] = np.ascontiguousarray(inp["w_in"])
    wdt = np.zeros((DEPTH, D, 36), np.float32)
    wdt[:, :, 0:4] = inp["w_in"][:, :, 3328:3332]
    wdt[:, :, 32:36] = inp["w_in"][:, :, 3328:3332]
    w["w_dt2"] = wdt
    w["w_out"] = np.ascontiguousarray(inp["w_out"])
    rgw = np.zeros((DEPTH, 2, 2, 128, 128), np.float32)
    for l in range(DEPTH):
        for gi, nm in enumerate(["rg_wa", "rg_wx"]):
            for c in range(2):
                for hh in range(2):
                    rgw[l, gi, c, hh * 64:(hh + 1) * 64, hh * 64:(hh + 1) * 64] = inp[nm][l, 2 * c + hh]
    w["rgw"] = rgw
    w["router_w"] = np.ascontiguousarray(inp["router_w"])
    w["router_b"] = np.ascontiguousarray(inp["router_b"])
    w["fng"] = np.ascontiguousarray(inp["final_norm_g"]).reshape(1, D)
    w.update(host_consts())
    if not moe:
        return w
    gu = inp["moe_w_gu"].reshape(DEPTH, NE, 8, 128, 8, 128, 2)
    gu = gu.transpose(0, 1, 4, 3, 2, 6, 5)
    w["gu"] = np.ascontiguousarray(gu).reshape(DEPTH, NE, 8, 128, 8 * 256)
    w["wd"] = np.ascontiguousarray(inp["moe_w_down"])
    bgu = inp["moe_b_gu"].reshape(DEPTH, NE, 8, 128, 2).transpose(0, 3, 1, 2, 4)
    w["bgu"] = np.ascontiguousarray(bgu).reshape(DEPTH, 128, NE * 16)
    w["bd"] = np.ascontiguousarray(inp["moe_b_down"])
    return w


def build(dbg=None, n_layers=DEPTH, stop=None):
    nc = bass.Bass("TRN2", target_bir_lowering=False)

    def din(name, shape, dt=F32):
        return nc.dram_tensor(name, list(shape), dt, kind="ExternalInput").ap()

    X = din("x", [T, D])
    PP = din("pp", [DEPTH, 128, NPP])
    W_IN = din("w_in", [DEPTH, D, DIN])
    W_DT2 = din("w_dt2", [DEPTH, D, 36])
    W_OUT = din("w_out", [DEPTH, D, D])
    RGW = din("rgw", [DEPTH, 2, 2, 128, 128])
    ROUTER_W = din("router_w", [DEPTH, D, NE])
    ROUTER_B = din("router_b", [DEPTH, NE])
    if stop in (None, "moe"):
        GU = din("gu", [DEPTH, NE, 8, 128, 2048])
        WD = din("wd", [DEPTH, NE, D, D])
        BGU = din("bgu", [DEPTH, 128, NE * 16])
        BD = din("bd", [DEPTH, NE, D])
    FNG = din("fng", [1, D])
    CONST = {k: din(k, v.shape) for k, v in host_consts().items()}
    OUT = nc.dram_tensor("out", [T, D], F32, kind="ExternalOutput").ap()
    HBUF = nc.dram_tensor("hbuf", [T, D], F32, kind="Internal").ap()
    NSLOT = NE * CAP
    XS = nc.dram_tensor("xs_buf", [NSLOT + 128, D], F32, kind="Internal").ap()
    YS = XS
    dbg_out = {}
    if dbg:
        for k, shp in dbg.items():
            dbg_out[k] = nc.dram_tensor("dbg_" + k, list(shp), F32, kind="ExternalOutput").ap()

    p = Prog(nc)
    PS = p.psum
    final_tokens = []

    def bank(i):
        return PS[:, i, :]

    ident = p.tile([128])
    identr = p.tile([128], F32R)
    trim = p.tile([128], F32R)
    onesr = p.tile([128], F32R)
    bonesr = p.tile([128], F32R)
    bones = p.tile([128])
    maskle = p.tile([128])
    negle = p.tile([128])
    e127 = p.tile([128])
    ppt = p.tile([DEPTH, NPP])
    for ap, nm, q in [(ident, "ident", "sp"), (bones, "bones", "sp"), (maskle, "maskle", "sp"),
                      (negle, "negle", "sp"), (e127, "e127", "sp"),
                      (identr, "ident", "pool"), (trim, "trim", "pool"), (onesr, "ones", "pool"),
                      (bonesr, "bones", "pool")]:
        p.dma(q, lambda e, ap=ap, nm=nm: e.dma_start(out=ap, in_=CONST[nm]), writes=["const"])
    p.dma("sp", lambda e: e.dma_start(out=ppt, in_=PP.rearrange("l p n -> p l n")), writes=["const"])
    p.barrier()

    for zi in range(NSLOT // 1024):
        p.dma("sp", lambda e, zi=zi: e.dma_start(out=XS[zi * 1024:(zi + 1) * 1024, :], in_=CONST["zeros"]), writes=["xsz"])
    p.dma("sp", lambda e: e.dma_start(out=XS[NSLOT:NSLOT + 128, :], in_=CONST["zeros"][0:128, :]), writes=["xsz"])

    bregs = {}

    def bnd(e):
        if "r" not in bregs:
            r = e.alloc_register("bnd")
            e.reg_mov(r, NSLOT - 1)
            bregs["r"] = r
        return bregs["r"]

    def dump(name, src_ap, reads, q="pool"):
        if name in dbg_out:
            tok = p.name("dbg")
            p.dma(q, lambda e: e.dma_start(out=dbg_out[name], in_=src_ap), reads=reads, writes=[tok, "dbgw_" + name])
            final_tokens.append(tok)

    def rms_tokens(src_dram, l, gcol, on_tile):
        m0 = p.mark()
        hr = [p.tile([D]) for _ in range(2)]
        xh = [p.tile([D]) for _ in range(2)]
        ss = p.tile([NT])
        rstd = p.tile([NT])
        for i in range(NT):
            ht, xt = hr[i % 2], xh[i % 2]
            htok, xtok = "hr%d" % (i % 2), "xh%d" % (i % 2)
            p.dma("sp", lambda e, ht=ht, i=i: e.dma_start(out=ht, in_=src_dram[i * 128:(i + 1) * 128, :]), writes=[htok])
            p.act(xt, ht, AF.Square, [htok], [xtok, "ss%d" % i], accum_out=ss[:, i:i + 1])
            p.act(rstd[:, i:i + 1], ss[:, i:i + 1], AF.Sqrt, ["ss%d" % i], ["rs%d" % i], bias=EPS, scale=1.0 / D)
            p.op("dve", lambda e, i=i: e.reciprocal(rstd[:, i:i + 1], rstd[:, i:i + 1]), ["rs%d" % i], ["rs%d" % i])
            p.act(xt, ht, AF.Copy, [htok, "rs%d" % i], [xtok], scale=rstd[:, i:i + 1])
            on_tile(i, xt, xtok, rstd[:, i:i + 1], ht, htok)
        return m0

    def transpose_tile(i, xt, xtok, dst, dsttok, gcols, banks, dst_is_bf16=True):
        for half in range(2):
            b = banks[half]
            btok = "pb%d" % b
            for kk in range(4):
                k = half * 4 + kk
                p.tr(PS[:, b, kk * 128:(kk + 1) * 128], xt[:, k * 128:(k + 1) * 128], ident,
                     [xtok, "const"], [btok], inc=(kk == 3))
            src = PS[:, b, :].rearrange("p (a b) -> p a b", b=128)
            gb = gcols[:, half * 4:half * 4 + 4].rearrange("p (a b) -> p a b", b=1).to_broadcast([128, 4, 128])
            p.tt("dve", dst[:, half * 4:half * 4 + 4, i * 128:(i + 1) * 128], src, gb, ALU.mult,
                 [btok, "const"], [dsttok])

    wring = {"n": 0}

    def load_w(l, col0, ncols, src=None):
        wt = p.tile([8, ncols], BF16)
        tok = p.name("w")
        s = (W_IN[l] if src is None else src).rearrange("(k p) c -> p k c", p=128)[:, :, col0:col0 + ncols]
        p.dma("pool", lambda e: e.dma_start(out=wt, in_=s), writes=[tok])
        return wt, tok

    bankrr = {"n": 0}

    def proj_fm(uT, wt, wtok, ncols, evac, banks, cbs=None):
        ncb = (ncols + 127) // 128
        for cb in (range(ncb) if cbs is None else cbs):
            cw = min(128, ncols - cb * 128)
            for tg in range(4):
                b = banks[bankrr["n"] % len(banks)]
                bankrr["n"] += 1
                btok = "pb%d" % b
                for k in range(8):
                    p.mm(PS[0:cw, b, :], wt[:, k, cb * 128:cb * 128 + cw], uT[:, k, tg * 512:(tg + 1) * 512],
                         k == 0, k == 7, ["uT", wtok], [btok], inc=(k == 7))
                evac(cb, tg, b, btok)

    def proj_tm(uT, wt, wtok, ncols, evac, banks):
        for i in range(NT):
            b = banks[bankrr["n"] % len(banks)]
            bankrr["n"] += 1
            btok = "pb%d" % b
            for k in range(8):
                p.mm(PS[:, b, 0:ncols], uT[:, k, i * 128:(i + 1) * 128], wt[:, k, :],
                     k == 0, k == 7, ["uT", wtok], [btok], inc=(k == 7))
            evac(i, b, btok)

    evrr = {"n": 0}

    def evac_copy(out, in_, reads, writes):
        eng = "act" if evrr["n"] % 2 == 0 else "dve"
        evrr["n"] += 1
        p.cp(eng, out, in_, reads, writes)

    def fm_rmsnorm(src, srctok, nch, lhs, joint, gcols, dst_c0, mixT, extra=None, extratok=None, banks=(6, 7)):
        m0 = p.mark()
        sq = [p.tile([512], F32R) for _ in range(2)]
        rs = [p.tile([512]) for _ in range(2)]
        n = 0
        for tg in range(4):
            sl = slice(tg * 512, (tg + 1) * 512)
            groups = [list(range(nch))] if joint else [[c] for c in range(nch)]
            for grp in groups:
                b = banks[n % 2]
                btok = "pb%d" % b
                for ci, c in enumerate(grp):
                    sqt = sq[n % 2]
                    sqtok = "nsq%d" % (n % 2)
                    p.act(sqt, src[:, c, sl], AF.Square, [srctok], [sqtok])
                    p.mm(PS[:, b, :], lhs, sqt, ci == 0, ci == len(grp) - 1, [sqtok, "const"], [btok])
                    n += 1
                rt = rs[n % 2]
                rtok = "nrs%d" % (n % 2)
                denom = 128.0 * len(grp) if joint else (128.0 if lhs is onesr else 64.0)
                p.act(rt, PS[:, b, :], AF.Sqrt, [btok], [rtok], bias=EPS, scale=1.0 / denom)
                p.op("dve", lambda e, rt=rt: e.reciprocal(rt, rt), [rtok], [rtok])
                for c in grp:
                    if extra is None:
                        p.stt("dve", mixT[:, dst_c0 + c, sl], src[:, c, sl], gcols[:, c:c + 1], rt, ALU.mult, ALU.mult,
                              [srctok, rtok, "const"], ["mixT"])
                    else:
                        p.stt("dve", src[:, c, sl], src[:, c, sl], gcols[:, c:c + 1], rt, ALU.mult, ALU.mult,
                              [srctok, rtok, "const"], [srctok])
                        p.tt("dve", mixT[:, dst_c0 + c, sl], src[:, c, sl], extra[:, c, sl], ALU.mult,
                             [srctok, extratok], ["mixT"])
        p.release(m0)

    src_h = X
    for l in range(n_layers):
        pl = ppt[:, l, :]
        lay0 = p.mark()
        gates_all = p.tile([NT, 4])
        slots_all = p.tile([NT, 4], I32)
        layB = p.mark()
        uT = p.tile([8, T], BF16)
        mixT = p.tile([8, T], BF16)

        def on_tile_p1(i, xt, xtok, rstd, ht, htok):
            transpose_tile(i, xt, xtok, uT, "uT", pl[:, 0:8], (0, 1) if i % 2 == 0 else (2, 3))
        m0 = rms_tokens(src_h, l, 0, on_tile_p1)
        p.release(m0)
        if l == 0:
            dump("uT", uT, ["uT"])
        if stop == "p1":
            break

        SKIP = os.environ.get('SKIPAB', '')
        if 'A' not in SKIP:
            m0 = p.mark()
            negmask = p.tile([4, 512], F32R)
            p.dma("pool", lambda e: e.dma_start(out=negmask, in_=CONST["negmask"]), writes=["negmask"])
            qT = p.tile([2, T], F32R)
            kT = p.tile([2, T], F32R)
            vtm = p.tile([NT, 256], F32R)
            oaT = p.tile([2, T])
            wq, wqtok = load_w(l, 0, 512)
            wv, wvtok = load_w(l, 512, 256)

            def ev_qk(cb, tg, b, btok):
                sl = slice(tg * 512, (tg + 1) * 512)
                if cb < 2:
                    p.act(qT[:, cb, sl], PS[:, b, :], AF.Copy, [btok], ["qT"], scale=0.125)
                else:
                    p.cp("dve", kT[:, cb - 2, sl], PS[:, b, :], [btok], ["kT"])
            proj_fm(uT, wq, wqtok, 512, ev_qk, (0, 1, 2, 3))

            def ev_v(i, b, btok):
                evac_copy(vtm[:, i, :], PS[:, b, 0:256], [btok], ["vtm"])
            proj_tm(uT, wv, wvtok, 256, ev_v, (0, 1, 2, 3))

            m1 = p.mark()
            Er = [p.tile([2, 512]) for _ in range(2)]
            SPr = [p.tile([2, 512], F32R) for _ in range(2)]
            SSr = [p.tile([2, 512], F32R) for _ in range(2)]
            ATr = [p.tile([2, 512], F32R) for _ in range(3)]
            items = []
            for sb in range(4):
                for c in range(2):
                    nkb = 4 * sb + 4
                    for kb in range(nkb - 1, -1, -1):
                        items.append((c, sb, kb, kb == nkb - 1, kb == 0, kb >= 4 * sb))
            NI = len(items)

            def zbanks(n):
                return 2 * (n % 3)

            def s1(n):
                c, sb, kb, first, last, diag = items[n]
                zb = zbanks(n)
                for hh in range(2):
                    ps = slice(hh * 64, (hh + 1) * 64)
                    p.mm(PS[:, zb + hh, :], kT[ps, c, kb * 128:(kb + 1) * 128], qT[ps, c, sb * 512:(sb + 1) * 512],
                         True, not diag, ["kT", "qT"], ["pb%d" % (zb + hh)], inc=(not diag))
                    if diag:
                        p.mm(PS[:, zb + hh, :], identr, negmask[:, kb - 4 * sb, :], False, True,
                             ["const", "negmask"], ["pb%d" % (zb + hh)])

            def s2(n):
                c, sb, kb, first, last, diag = items[n]
                zb = zbanks(n)
                ztoks = ["pb%d" % zb, "pb%d" % (zb + 1)]
                z2 = PS[:, zb:zb + 2, :]
                p.act(Er[n % 2], z2, AF.Exp, ztoks, ["E%d" % (n % 2)])
                p.act(SPr[n % 2], Er[n % 2], AF.Ln, ["E%d" % (n % 2)], ["SP%d" % (n % 2)], bias=1.0)

            def s3(n):
                c, sb, kb, first, last, diag = items[n]
                zb = zbanks(n)
                for hh in range(2):
                    p.mm(PS[:, zb + hh, :], trim, SPr[n % 2][:, hh, :], False, first,
                         ["SP%d" % (n % 2), "const"], ["pb%d" % (zb + hh)], inc=first)
                    if not first:
                        p.mm(PS[:, zb + hh, :], onesr, SSr[(n - 1) % 2][:, hh, :], False, True,
                             ["SS%d" % ((n - 1) % 2), "const"], ["pb%d" % (zb + hh)])
                if not last:
                    if first:
                        p.ts("dve", SSr[n % 2], SPr[n % 2], -1.0, None, ALU.mult, None, ["SP%d" % (n % 2)], ["SS%d" % (n % 2)])
                    else:
                        p.tt("dve", SSr[n % 2], SSr[(n - 1) % 2], SPr[n % 2], ALU.subtract,
                             ["SS%d" % ((n - 1) % 2), "SP%d" % (n % 2)], ["SS%d" % (n % 2)])

            def s4(n):
                zb = zbanks(n)
                p.act(ATr[n % 3], PS[:, zb:zb + 2, :], AF.Exp, ["pb%d" % zb, "pb%d" % (zb + 1)], ["AT%d" % (n % 3)])

            def s5(n):
                c, sb, kb, first, last, diag = items[n]
                for hh in range(2):
                    p.mm(PS[:, 6 + hh, :], vtm[:, kb, c * 128:(c + 1) * 128], ATr[n % 3][:, hh, :], first, last,
                         ["vtm", "AT%d" % (n % 3)], ["pb%d" % (6 + hh)])
                if last:
                    for hh in range(2):
                        ps = slice(hh * 64, (hh + 1) * 64)
                        p.cp("dve", oaT[ps, c, sb * 512:(sb + 1) * 512], PS[ps, 6 + hh, :], ["pb%d" % (6 + hh)], ["oaT"])

            for n in range(NI + 2):
                if n < NI:
                    s1(n)
                    s2(n)
                if 1 <= n <= NI:
                    s3(n - 1)
                    s4(n - 1)
                if 2 <= n <= NI + 1:
                    s5(n - 2)
            p.release(m1)
            if l == 0:
                dump("oaT", oaT, ["oaT"])
            fm_rmsnorm(oaT, "oaT", 2, onesr, True, pl[:, 16:18], 0, mixT, banks=(0, 1))
            p.release(m0)
            if l == 0:
                dump("mixT", mixT, ["mixT"])
            if stop == "A":
                break

            m0 = p.mark()
            obT = p.tile([2, T])
            rgwt = p.tile([4, 128], F32R)
            p.dma("pool", lambda e: e.dma_start(out=rgwt, in_=RGW[l].rearrange("g c i j -> i (g c) j")), writes=["rgw"])
            nsl = p.tile([2])
            nsl2 = p.tile([2])
            p.act(nsl, pl[:, 38:40], AF.Exp, ["const"], ["nsl"], scale=-1.0)
            p.act(nsl, nsl, AF.Ln, ["nsl"], ["nsl"], bias=1.0)
            p.ts("dve", nsl, nsl, -8.0, None, ALU.mult, None, ["nsl"], ["nsl"])
            p.ts("dve", nsl2, nsl, 2.0, None, ALU.mult, None, ["nsl"], ["nsl2"])
            for c in range(2):
                m1 = p.mark()
                wx, wxtok = load_w(l, 768 + c * 128, 128)
                wg, wgtok = load_w(l, 1024 + c * 128, 128)
                xp = p.tile([T + 3])
                xcR = p.tile([T], F32R); xc = xcR.bitcast(F32)
                gtR = p.tile([T], F32R); gt = gtR.bitcast(F32)
                rR = p.tile([T], F32R); rr = rR.bitcast(F32)
                igR = p.tile([T], F32R); ig = igR.bitcast(F32)
                sR = p.tile([T], F32R); s_ = sR.bitcast(F32)
                at = p.tile([T])
                mt = p.tile([T])
                X = "B_"
                p.memset("pool", xp[:, 0:3], 0.0, [X + "xp"])

                def ev_x(cb, tg, b, btok, xp=xp):
                    evac_copy(xp[:, 3 + tg * 512:3 + (tg + 1) * 512], PS[:, b, :], [btok], [X + "xp"])
                proj_fm(uT, wx, wxtok, 128, ev_x, (0, 1, 2, 3))

                def ev_g(cb, tg, b, btok, gtR=gtR):
                    evac_copy(gtR[:, tg * 512:(tg + 1) * 512], PS[:, b, :], [btok], [X + "gt"])
                proj_fm(uT, wg, wgtok, 128, ev_g, (0, 1, 2, 3))
                cw = 24 + c * 4
                p.ts("dve", xcR, xp[:, 3:3 + T], pl[:, cw + 3:cw + 4], pl[:, 32 + c:33 + c], ALU.mult, ALU.add,
                     [X + "xp", "const"], [X + "xc"])
                for k in range(3):
                    p.stt("dve", xcR, xp[:, k:k + T], pl[:, cw + k:cw + k + 1], xc, ALU.mult, ALU.add,
                          [X + "xp", X + "xc", "const"], [X + "xc"])
                for tg in range(4):
                    sl = slice(tg * 512, (tg + 1) * 512)
                    for gi, (dst, bcol, tokn) in enumerate([(rR, 34, "r"), (igR, 36, "ig")]):
                        b = 4 + (2 * tg + gi) % 4
                        p.mm(PS[:, b, :], rgwt[:, gi * 2 + c, :], xcR[:, sl], True, True, [X + "xc", "rgw"], ["pb%d" % b])
                        p.act(dst[:, sl], PS[:, b, :], AF.Sigmoid, ["pb%d" % b, "const"], [X + tokn],
                              bias=pl[:, bcol + c:bcol + c + 1])
                p.act(at, rr, AF.Exp, [X + "r", "nsl"], [X + "a"], scale=nsl[:, c:c + 1])
                p.act(mt, rr, AF.Exp, [X + "r", "nsl2"], [X + "m"], scale=nsl2[:, c:c + 1])
                p.act(mt, mt, AF.Relu, [X + "m"], [X + "m"], scale=-1.0, bias=1.0)
                p.act(mt, mt, AF.Sqrt, [X + "m"], [X + "m"])
                p.memset("dve", mt[:, 0:1], 1.0, [X + "m"])
                p.tt("dve", mt, mt, ig, ALU.mult, [X + "m", X + "ig"], [X + "m"])
                p.tt("dve", mt, mt, xc, ALU.mult, [X + "m", X + "xc"], [X + "m"])
                p.op("dve", lambda e, c=c, at=at, mt=mt: e.tensor_tensor_scan(obT[:, c, :], at, mt, 0.0, ALU.mult, ALU.add),
                     [X + "a", X + "m"], ["obT"])
                p.act(sR, gt, AF.Square, [X + "gt"], [X + "s"])
                p.ts("dve", sR, s_, 0.044715, 1.0, ALU.mult, ALU.add, [X + "s"], [X + "s"])
                p.tt("dve", sR, s_, gt, ALU.mult, [X + "s", X + "gt"], [X + "s"])
                p.act(sR, s_, AF.Sigmoid, [X + "s"], [X + "s"], scale=1.5957691216)
                p.tt("dve", sR, s_, gt, ALU.mult, [X + "s", X + "gt"], [X + "s"])
                p.tt("dve", obT[:, c, :], obT[:, c, :], s_, ALU.mult, ["obT", X + "s"], ["obT"])
                p.release(m1)
            fm_rmsnorm(obT, "obT", 2, onesr, True, pl[:, 18:20], 2, mixT, banks=(0, 1))
            p.release(m0)
            if l == 0:
                dump("mixT", mixT, ["mixT"])
            if stop == "B":
                break

        if 'C' not in SKIP:
            m0 = p.mark()
            X = "C_"
            NC2 = 32
            lb = p.tile([2])
            oml = p.tile([2])
            ones128 = p.tile([128])
            mask64 = p.tile([2, 64])
            p.dma("sp", lambda e: e.dma_start(out=mask64, in_=CONST["mask64"]), writes=[X + "mask"])
            p.memset("pool", ones128, 1.0, [X + "ones"])
            if l == 0:
                p.memset("dve", lb, 0.0, [X + "lb"])
            else:
                p.tt("dve", lb, pl[:, 42:44], pl[:, 40:42], ALU.subtract, ["const"], [X + "lb"])
                p.act(lb, lb, AF.Sigmoid, [X + "lb"], [X + "lb"])
            p.ts("dve", oml, lb, -1.0, 1.0, ALU.mult, ALU.add, [X + "lb"], [X + "oml"])
            for c in range(2):
                m1 = p.mark()
                T1 = p.tile([T]); T2 = p.tile([T]); T3 = p.tile([T]); T4 = p.tile([T]); T5 = p.tile([T])
                T6R = p.tile([T], F32R); T7R = p.tile([T], F32R); T8R = p.tile([T], F32R)
                khatR = p.tile([NT, 128], F32R)
                vvzR = p.tile([NT, 2, 128], F32R)
                ST2R = p.tile([NC2 + 1, 128], F32R)
                ST2 = ST2R.bitcast(F32)
                scmR = [p.tile([2, 64], F32R) for _ in range(2)]
                dl = p.tile([NC2])
                utmp = [p.tile([128]) for _ in range(2)]
                v3 = lambda a: a.rearrange("p (n q) -> p n q", q=64)
                p.ts("pool", vvzR.rearrange("p a b c -> p (a b) c"), ones128.rearrange("p (a b) -> p a b", a=1).to_broadcast([128, 2 * NT, 128]),
                     0.0, None, ALU.mult, None, [X + "ones"], [X + "vvz"])
                p.ts("pool", ST2R[:, 0, :], ones128, 0.0, None, ALU.mult, None, [X + "ones"], [X + "st0"])
                mw = p.mark()
                wq_, wqt = load_w(l, 1280 + c * 128, 128)
                wf_, wft = load_w(l, 1536 + c * 128, 128)
                wi_, wit = load_w(l, 1792 + c * 128, 128)

                def ev_q(cb, tg, b, btok, T1=T1):
                    p.act(T1[:, tg * 512:(tg + 1) * 512], PS[:, b, :], AF.Silu, [btok], [X + "T1"])
                proj_fm(uT, wq_, wqt, 128, ev_q, (0, 1, 2, 3))

                def ev_f(cb, tg, b, btok, T2=T2):
                    p.act(T2[:, tg * 512:(tg + 1) * 512], PS[:, b, :], AF.Sigmoid, [btok], [X + "T2"])
                proj_fm(uT, wf_, wft, 128, ev_f, (0, 1, 2, 3))

                def ev_i(i, b, btok, vvzR=vvzR):
                    p.cp("dve", vvzR[:, i, 0, 0:64], PS[:, b, 0:64], [btok], [X + "vvz"])
                    p.cp("dve", vvzR[:, i, 1, 64:128], PS[:, b, 64:128], [btok], [X + "vvz"])
                proj_tm(uT, wi_, wit, 128, ev_i, (4, 5, 6, 7))
                p.release(mw)
                p.ts("dve", T2, T2, oml[:, c:c + 1], lb[:, c:c + 1], ALU.mult, ALU.add, [X + "T2", X + "oml", X + "lb"], [X + "T2"])
                p.act(T3, T2, AF.Ln, [X + "T2"], [X + "T3"])
                p.ts("dve", T2, T2, -1.0, 1.0, ALU.mult, ALU.add, [X + "T2"], [X + "T2"])
                for n in range(NC2):
                    sl = slice(n * 64, (n + 1) * 64)
                    p.op("dve", lambda e, sl=sl, T4=T4, T3=T3: e.tensor_tensor_scan(T4[:, sl], ones128[:, 0:64], T3[:, sl], 0.0, ALU.mult, ALU.add),
                         [X + "T3", X + "ones"], [X + "T4"])
                b3 = v3(T4)
                p.tt("dve", v3(T3), b3, b3[:, :, 31:32].to_broadcast([128, NC2, 64]), ALU.subtract, [X + "T4"], [X + "T3"])
                p.ts("dve", T5, T3, 40.0, None, ALU.min, None, [X + "T3"], [X + "T5"])
                p.act(T5, T5, AF.Exp, [X + "T5"], [X + "T5"])
                p.tt("dve", T6R, T1, T5, ALU.mult, [X + "T1", X + "T5"], [X + "T6"])
                p.ts("dve", T5, T3, -40.0, None, ALU.max, None, [X + "T3"], [X + "T5"])
                p.act(T5, T5, AF.Exp, [X + "T5"], [X + "T5"], scale=-1.0)
                p.tt("dve", T7R, T2, T5, ALU.mult, [X + "T2", X + "T5"], [X + "T7"])
                p.act(T5, T4, AF.Exp, [X + "T4"], [X + "T5"])
                p.tt("dve", T8R, T1, T5, ALU.mult, [X + "T1", X + "T5"], [X + "T8"])
                p.tt("dve", v3(T3), b3[:, :, 63:64].to_broadcast([128, NC2, 64]), b3, ALU.subtract, [X + "T4"], [X + "T3"])
                p.act(T5, T3, AF.Exp, [X + "T3"], [X + "T5"])
                p.tt("dve", T3, T2, T5, ALU.mult, [X + "T2", X + "T5"], [X + "T3"])
                p.act(dl, b3[:, :, 63], AF.Exp, [X + "T4"], [X + "dl"])
                for n4 in range(4):
                    b = 4 + n4 % 2
                    for j in range(4):
                        n = n4 * 4 + j
                        p.tr(PS[:, b, j * 128:(j + 1) * 128], T3[:, n * 128:(n + 1) * 128], ident, [X + "T3", "const"], ["pb%d" % b], inc=(j == 3))
                    p.cp("act", khatR[:, n4 * 4:n4 * 4 + 4, :], PS[:, b, :].rearrange("p (a b) -> p a b", b=128), ["pb%d" % b], [X + "khat"])
                CS = int(os.environ.get('CSTOP', '9'))
                for g8 in (range(4) if CS >= 5 else []):
                    for j in range(8):
                        n = g8 * 8 + j
                        b = (g8 % 2) * 2 + j % 2
                        reg = PS[:, b, (j // 2) * 128:(j // 2 + 1) * 128]
                        pr = slice((n % 2) * 64, (n % 2) * 64 + 64)
                        p.mm(reg, khatR[pr, n // 2, :], vvzR[pr, n // 2, 0, :], True, False, [X + "khat", X + "vvz"], ["pb%d" % b], inc=False)
                        p.mm(reg, khatR[pr, n // 2, :], vvzR[pr, n // 2, 1, :], False, True, [X + "khat", X + "vvz"], ["pb%d" % b], inc=(j >= 6))
                    for j in (range(8) if os.environ.get('CSUB', 'r') == 'r' else []):
                        n = g8 * 8 + j
                        b = (g8 % 2) * 2 + j % 2
                        reg = PS[:, b, (j // 2) * 128:(j // 2 + 1) * 128]
                        ut = utmp[n % 2]
                        p.tt("dve", ut, reg, bones, ALU.mult, ["pb%d" % b, "const"], [X + "ut%d" % (n % 2)])
                        p.stt("dve", ST2R[:, n + 1, :], ST2[:, n, :], dl[:, n:n + 1], ut, ALU.mult, ALU.add,
                              [X + "st%d" % n, X + "dl", X + "ut%d" % (n % 2)], [X + "st%d" % (n + 1)])
                for n in (range(NC2) if CS >= 6 else []):
                    tl = slice((n // 2) * 128, (n // 2 + 1) * 128)
                    sl = slice(n * 64, (n + 1) * 64)
                    sb_ = 2 * (n % 2)
                    for hh in range(2):
                        ps_ = slice(hh * 64, (hh + 1) * 64)
                        p.mm(PS[:, sb_ + hh, 0:64], T7R[ps_, tl], T6R[ps_, sl], True, True,
                             [X + "T7", X + "T6"], ["pb%d" % (sb_ + hh)], inc=(hh == 1))
                    sc = scmR[n % 2]
                    p.tt("dve", sc, PS[:, sb_:sb_ + 2, 0:64],
                         mask64[:, n % 2:n % 2 + 1, :].to_broadcast([128, 2, 64]), ALU.mult,
                         ["pb%d" % sb_, "pb%d" % (sb_ + 1), X + "mask"], [X + "scm%d" % (n % 2)])
                    ob = 6 + (n // 8) % 2
                    reg = PS[:, ob, (n % 8) * 64:(n % 8 + 1) * 64]
                    p.mm(reg, vvzR[:, n // 2, 0, :], sc[:, 0, :], True, False, [X + "vvz", X + "scm%d" % (n % 2)], ["pb%d" % ob], inc=False)
                    p.mm(reg, vvzR[:, n // 2, 1, :], sc[:, 1, :], False, False, [X + "vvz", X + "scm%d" % (n % 2)], ["pb%d" % ob], inc=False)
                    p.mm(reg, ST2R[:, n, :], T8R[:, sl], False, True, [X + "st%d" % n, X + "T8"], ["pb%d" % ob], inc=True)
                    if n % 8 == 7:
                        p.cp("act", T4[:, (n - 7) * 64:(n + 1) * 64], PS[:, ob, :], ["pb%d" % ob, X + "dl", X + "T5"], [X + "oc"])
                mw = p.mark()
                wg_, wgt = load_w(l, 2048 + c * 128, 128)

                def ev_g2(cb, tg, b, btok, T1=T1):
                    p.act(T1[:, tg * 512:(tg + 1) * 512], PS[:, b, :], AF.Silu, [btok, X + "T8", X + "T6"], [X + "sg"])
                proj_fm(uT, wg_, wgt, 128, ev_g2, (0, 1, 2, 3))
                p.release(mw)
                fm_rmsnorm(T4.rearrange("p (a b) -> p a b", a=1), X + "oc", 1, bonesr, False, pl[:, 20 + c:21 + c], 4 + c, mixT,
                           extra=T1.rearrange("p (a b) -> p a b", a=1), extratok=X + "sg", banks=(4, 5))
                p.release(m1)
            p.release(m0)
            if l == 0:
                dump("mixC", mixT[:, 4:6, :], ["mixT"])
                if not SKIP:
                    dump("mixT", mixT, ["mixT"])
            if stop == "C":
                break

        if 'D' not in SKIP:
            m0 = p.mark()
            X = "D_"
            selh = p.tile([4, 128])
            selg = p.tile([2, 128])
            p.dma("sp", lambda e: e.dma_start(out=selh[0:36], in_=CONST["selh"]), writes=[X + "sel"])
            p.dma("sp", lambda e: e.dma_start(out=selg[0:36], in_=CONST["selg"]), writes=[X + "sel"])
            Fdt = p.tile([T])
            Fa = p.tile([T])
            dta = p.tile([NT, 8])
            alb = p.tile([NT, 4])
            edl = p.tile([NT, 4])
            fs = p.tile([NT, 4])
            nA = p.tile([1])
            ones128 = p.tile([128])
            p.memset("pool", ones128, 1.0, [X + "ones"])
            p.memset("pool", Fa[0:32, :], 0.0, [X + "Fa"])
            mw = p.mark()
            wdt = p.tile([8, 36], BF16)
            p.dma("pool", lambda e: e.dma_start(out=wdt, in_=W_DT2[l].rearrange("(k p) c -> p k c", p=128)), writes=[X + "wdt"])
            for tg in range(4):
                sl = slice(tg * 512, (tg + 1) * 512)
                b = tg % 2
                for k in range(8):
                    p.mm(PS[0:36, b, :], wdt[:, k, :], uT[:, k, sl], k == 0, k == 7, ["uT", X + "wdt"], ["pb%d" % b], inc=(k == 7))
                p.act(Fdt[0:36, sl], PS[0:36, b, :], AF.Exp, ["pb%d" % b, "const"], [X + "Fdt"], bias=pl[0:36, 76:77])
            p.act(Fdt[0:36, :], Fdt[0:36, :], AF.Ln, [X + "Fdt"], [X + "Fdt"], bias=1.0)
            p.release(mw)
            p.act(nA, pl[:, 77:78], AF.Exp, ["const"], [X + "nA"])
            p.ts("dve", nA, nA, -1.0, None, ALU.mult, None, [X + "nA"], [X + "nA"])
            p.ts("dve", Fdt[32:36, :], Fdt[32:36, :], nA[32:36, :], None, ALU.mult, None, [X + "Fdt", X + "nA"], [X + "Fdt"])
            for n in range(NT):
                sl = slice(n * 128, (n + 1) * 128)
                p.op("dve", lambda e, sl=sl: e.tensor_tensor_scan(Fa[32:36, sl], ones128[32:36, :], Fdt[32:36, sl], 0.0, ALU.mult, ALU.add),
                     [X + "Fdt", X + "ones"], [X + "Fa"])
            for n in range(NT):
                sl = slice(n * 128, (n + 1) * 128)
                p.tr(PS[:, 0, n * 4:n * 4 + 4], Fdt[0:4, sl], ident[0:4, 0:4], [X + "Fdt", "const"], ["pb0"], inc=False)
                p.tr(PS[:, 2, n * 4:n * 4 + 4], Fa[32:36, sl], ident[32:36, 32:36], [X + "Fa", "const"], ["pb2"], inc=True)
            p.cp("dve", dta[:, :, 0:4], PS[:, 0, 0:64].rearrange("p (a b) -> p a b", b=4), ["pb0"], [X + "dta"])
            p.cp("dve", dta[:, :, 4:8], PS[:, 2, 0:64].rearrange("p (a b) -> p a b", b=4), ["pb2"], [X + "dta"])
            p.mm(PS[:, 1, 0:64], e127, dta[:, :, 4:8], True, True, [X + "dta", "const"], ["pb1"])
            p.cp("dve", alb, PS[:, 1, 0:64].rearrange("p (a b) -> p a b", b=4), ["pb1"], [X + "alb"])
            p.act(edl, alb, AF.Exp, [X + "alb"], [X + "edl"])
            p.tt("dve", fs, alb, dta[:, :, 4:8], ALU.subtract, [X + "alb", X + "dta"], [X + "fs"])
            p.act(fs, fs, AF.Exp, [X + "fs"], [X + "fs"])
            p.tt("dve", fs, fs, dta[:, :, 0:4], ALU.mult, [X + "fs", X + "dta"], [X + "fs"])
            for g in range(2):
                m1 = p.mark()
                EGR = p.tile([T], F32R); EG = EGR.bitcast(F32)
                cR = [p.tile([T], F32R) for _ in range(3)]
                cF = [a.bitcast(F32) for a in cR]
                xdzR = p.tile([NT, 2, 128], F32R)
                xddR = p.tile([NT, 128], F32R)
                BtmR = p.tile([NT, 128], F32R)
                prevR = p.tile([NT + 1, 128], F32R); prev = prevR.bitcast(F32)
                MtR = [p.tile([2, 128], F32R) for _ in range(2)]
                xp = p.tile([T + 3])
                cv = p.tile([T])
                Dt = [p.tile([2, 128]) for _ in range(2)]
                Lm = [p.tile([2, 128]) for _ in range(2)]
                ytmp = [p.tile([512]) for _ in range(2)]
                ptmp = [p.tile([128]) for _ in range(2)]
                for tg in range(4):
                    sl = slice(tg * 512, (tg + 1) * 512)
                    b = 2 + tg % 2
                    p.mm(PS[:, b, :], selg[0:36, g, :], Fa[0:36, sl], True, True, [X + "Fa", X + "sel"], ["pb%d" % b])
                    p.act(EGR[:, sl], PS[:, b, :], AF.Exp, ["pb%d" % b], [X + "EG"])
                p.ts("pool", xdzR.rearrange("p a b c -> p (a b) c"), ones128.rearrange("p (a b) -> p a b", a=1).to_broadcast([128, 2 * NT, 128]),
                     0.0, None, ALU.mult, None, [X + "ones"], [X + "xdz"])
                p.ts("pool", prevR[:, 0, :], ones128, 0.0, None, ALU.mult, None, [X + "ones"], [X + "pv0"])
                p.memset("pool", xp[:, 0:3], 0.0, [X + "xp"])
                for ci, ch in enumerate([g, 2 + g, 4 + g]):
                    mw = p.mark()
                    wc, wctok = load_w(l, 2560 + ch * 128, 128)

                    def ev_c(cb, tg, b, btok):
                        evac_copy(xp[:, 3 + tg * 512:3 + (tg + 1) * 512], PS[:, b, :], [btok], [X + "xp"])
                    proj_fm(uT, wc, wctok, 128, ev_c, (4, 5, 6, 7))
                    p.release(mw)
                    cw = 44 + ch * 4
                    p.ts("dve", cv, xp[:, 3:3 + T], pl[:, cw + 3:cw + 4], pl[:, 68 + ch:69 + ch], ALU.mult, ALU.add,
                         [X + "xp", "const"], [X + "cv"])
                    for k in range(3):
                        p.stt("dve", cv, xp[:, k:k + T], pl[:, cw + k:cw + k + 1], cv, ALU.mult, ALU.add,
                              [X + "xp", X + "cv", "const"], [X + "cv"])
                    p.act(cR[ci], cv, AF.Silu, [X + "cv"], [X + "c%d" % ci])
                for n in range(NT):
                    sl = slice(n * 128, (n + 1) * 128)
                    b = n % 2
                    p.tr(PS[:, b, 0:128], cF[0][:, sl], ident, [X + "c0", "const"], ["pb%d" % b], inc=False)
                    p.tr(PS[:, b, 128:256], cF[1][:, sl], ident, [X + "c1", "const"], ["pb%d" % b], inc=True)
                    for hh in range(2):
                        h = 2 * g + hh
                        cs = slice(hh * 64, (hh + 1) * 64)
                        p.ts("dve", xdzR[:, n, hh, cs], PS[:, b, cs], dta[:, n, h:h + 1], None, ALU.mult, None,
                             ["pb%d" % b, X + "dta"], [X + "xdz"])
                        p.ts("dve", xddR[:, n, cs], PS[:, b, cs], fs[:, n, h:h + 1], None, ALU.mult, None,
                             ["pb%d" % b, X + "fs"], [X + "xdd"])
                    p.cp("act", BtmR[:, n, :], PS[:, b, 128:256], ["pb%d" % b], [X + "Btm"])
                for n in range(NT):
                    b = 4 + n // 4
                    reg = PS[:, b, (n % 4) * 128:(n % 4 + 1) * 128]
                    p.mm(reg, BtmR[:, n, :], xddR[:, n, :], True, True, [X + "Btm", X + "xdd"], ["pb%d" % b], inc=(n % 4 == 3))
                for n in range(NT):
                    b = 4 + n // 4
                    reg = PS[:, b, (n % 4) * 128:(n % 4 + 1) * 128]
                    pt = ptmp[n % 2]
                    p.tt("dve", pt.rearrange("p (a b) -> p a b", b=64), prev[:, n, :].rearrange("p (a b) -> p a b", b=64),
                         edl[:, n, 2 * g:2 * g + 2].rearrange("p (a b) -> p a b", b=1).to_broadcast([128, 2, 64]), ALU.mult,
                         [X + "pv%d" % n, X + "edl"], [X + "pt%d" % (n % 2)])
                    p.tt("dve", prevR[:, n + 1, :], pt, reg, ALU.add, [X + "pt%d" % (n % 2), "pb%d" % b], [X + "pv%d" % (n + 1)])
                mw = p.mark()
                wz, wztok = load_w(l, 2304 + g * 128, 128)

                def ev_z(cb, tg, b, btok):
                    p.act(cv[:, tg * 512:(tg + 1) * 512], PS[:, b, :], AF.Silu, [btok, X + "c2"], [X + "zt"])
                proj_fm(uT, wz, wztok, 128, ev_z, (0, 1))
                p.release(mw)
                yT = xp[:, 0:T]
                for n in range(NT):
                    sl = slice(n * 128, (n + 1) * 128)
                    lb_ = 2 + n % 2
                    for hh in range(2):
                        p.mm(PS[:, lb_, hh * 128:(hh + 1) * 128], selh[0:36, 2 * g + hh, :], Fa[0:36, sl], True, True,
                             [X + "Fa", X + "sel"], ["pb%d" % lb_], inc=False)
                    gb_ = 6 + n % 2
                    p.mm(PS[:, gb_, 0:128], cR[1][:, sl], cR[2][:, sl], True, True, [X + "c1", X + "c2"], ["pb%d" % gb_], inc=True)
                    dt_, lm_, mt_ = Dt[n % 2], Lm[n % 2], MtR[n % 2]
                    for hh in range(2):
                        h = 2 * g + hh
                        p.stt("dve", dt_[:, hh, :], PS[:, lb_, hh * 128:(hh + 1) * 128], dta[:, n, 4 + h:5 + h], negle, ALU.subtract, ALU.add,
                              ["pb%d" % lb_, X + "dta", "const"], [X + "Dt%d" % (n % 2)])
                    p.act(lm_, dt_, AF.Exp, [X + "Dt%d" % (n % 2)], [X + "Lm%d" % (n % 2)])
                    p.tt("dve", mt_, lm_, PS[:, gb_, 0:128].rearrange("p (a b) -> p a b", a=1).to_broadcast([128, 2, 128]), ALU.mult,
                         [X + "Lm%d" % (n % 2), "pb%d" % gb_], [X + "Mt%d" % (n % 2)])
                    yb = (n // 4) % 2
                    db, ob = yb, 4 + yb
                    reg_d = PS[:, db, (n % 4) * 128:(n % 4 + 1) * 128]
                    reg_o = PS[:, ob, (n % 4) * 128:(n % 4 + 1) * 128]
                    p.mm(reg_d, xdzR[:, n, 0, :], mt_[:, 0, :], True, False, [X + "xdz", X + "Mt%d" % (n % 2)], ["pb%d" % db], inc=False)
                    p.mm(reg_d, xdzR[:, n, 1, :], mt_[:, 1, :], False, True, [X + "xdz", X + "Mt%d" % (n % 2)], ["pb%d" % db], inc=False)
                    p.mm(reg_o, prevR[:, n, :], cR[2][:, sl], True, True, [X + "pv%d" % n, X + "c2"], ["pb%d" % ob], inc=True)
                    if n % 4 == 3:
                        s4 = slice((n - 3) * 128, (n + 1) * 128)
                        yt = ytmp[yb]
                        p.tt("dve", yt, PS[:, ob, :], EG[:, s4], ALU.mult, ["pb%d" % ob, "pb%d" % db, X + "EG"], [X + "yt%d" % yb])
                        p.tt("dve", yT[:, s4], yt, PS[:, db, :], ALU.add, [X + "yt%d" % yb, "pb%d" % db, X + "cv", X + "xp"], [X + "y"])
                p.stt("dve", yT, cF[0], pl[:, 74 + g:75 + g], yT, ALU.mult, ALU.add, [X + "c0", X + "y", "const"], [X + "y"])
                p.tt("dve", yT, yT, cv, ALU.mult, [X + "y", X + "zt"], [X + "y"])
                fm_rmsnorm(yT.rearrange("p (a b) -> p a b", a=1), X + "y", 1, onesr, True, pl[:, 22 + g:23 + g], 6 + g, mixT, banks=(6, 7))
                p.release(m1)
            p.release(m0)
            if l == 0:
                dump("mixD", mixT[:, 6:8, :], ["mixT"])
                if not SKIP:
                    dump("mixT", mixT, ["mixT"])
            if stop == "D":
                break
        if SKIP:
            p.memset('dve', mixT, 0.0, ['mixT'])

        m0 = p.mark()
        X = "M_"
        wo = p.tile([8, D], BF16)
        p.dma("pool", lambda e: e.dma_start(out=wo, in_=W_OUT[l].rearrange("(k p) c -> p k c", p=128)), writes=[X + "wo"])
        rw = p.tile([8, NE])
        p.dma("sp", lambda e: e.dma_start(out=rw, in_=ROUTER_W[l].rearrange("(k p) c -> p k c", p=128)), writes=[X + "rw"])
        rb = p.tile([NE])
        p.dma("sp", lambda e: e.dma_start(out=rb, in_=ROUTER_B[l:l + 1, :].partition_broadcast(128)), writes=[X + "rb"])
        iota32 = p.tile([NE])
        p.dma("sp", lambda e: e.dma_start(out=iota32, in_=CONST["iota32"]), writes=[X + "iota"])
        strib = p.tile([128], BF16)
        onesb = p.tile([128], BF16)
        p.dma("pool", lambda e: e.dma_start(out=strib, in_=CONST["stri"]), writes=[X + "cb"])
        p.dma("pool", lambda e: e.dma_start(out=onesb, in_=CONST["ones"]), writes=[X + "cb"])
        cnt = p.tile([NE])
        p.memset("dve", cnt, 0.0, [X + "cnt"])
        mP = p.mark()
        hr = [p.tile([D]) for _ in range(2)]
        xh = [p.tile([D]) for _ in range(3)]
        xnT = [p.tile([8, 128]) for _ in range(2)]
        ss = p.tile([NT]); rstd = p.tile([NT])
        lg = p.tile([NE]); mx = p.tile([8]); mxi = p.tile([8], U32); idf = p.tile([8])
        nm0 = p.tile([1]); ex = p.tile([4]); esum = p.tile([1])
        maskb = p.tile([NE], BF16)
        eq = p.tile([NE]); posk = p.tile([4]); big = p.tile([4]); slf = p.tile([4])
        for i in range(int(os.environ.get("NTP7", NT))):
            ht, xt, xT_ = hr[i % 2], xh[i % 3], xnT[i % 2]
            htok, xtok, xTtok = X + "hr%d" % (i % 2), X + "xh%d" % (i % 3), X + "xT%d" % (i % 2)
            rows = slice(i * 128, (i + 1) * 128)
            p.dma("sp", lambda e, ht=ht, rows=rows: e.dma_start(out=ht, in_=src_h[rows, :]), reads=["hbuf%d" % i], writes=[htok])
            for half in range(2):
                b = half
                for k in range(8):
                    p.mm(PS[:, b, :], mixT[:, k, rows], wo[:, k, half * 512:(half + 1) * 512], k == 0, k == 7,
                         ["mixT", X + "wo"], ["pb%d" % b], inc=(k == 7))
                p.tt("dve", ht[:, half * 512:(half + 1) * 512], ht[:, half * 512:(half + 1) * 512], PS[:, b, :], ALU.add,
                     [htok, "pb%d" % b], [htok])
            p.dma("sp", lambda e, ht=ht, rows=rows: e.dma_start(out=HBUF[rows, :], in_=ht), reads=[htok], writes=["hbuf%d" % i])
            if l == 0 and i == 0:
                pass
            p.act(xt, ht, AF.Square, [htok], [xtok, X + "ss%d" % i], accum_out=ss[:, i:i + 1])
            p.act(rstd[:, i:i + 1], ss[:, i:i + 1], AF.Sqrt, [X + "ss%d" % i], [X + "rs%d" % i], bias=EPS, scale=1.0 / D)
            p.op("dve", lambda e, i=i: e.reciprocal(rstd[:, i:i + 1], rstd[:, i:i + 1]), [X + "rs%d" % i], [X + "rs%d" % i])
            p.act(xt, ht, AF.Copy, [htok, X + "rs%d" % i], [xtok], scale=rstd[:, i:i + 1])
            transpose_tile(0, xt, xtok, xT_, xTtok, pl[:, 8:16], (2, 3))
            for k in range(8):
                p.mm(PS[:, 4, 0:NE], xT_[:, k, :], rw[:, k, :], k == 0, k == 7, [xTtok, X + "rw"], ["pb4"], inc=(k == 7))
            p.tt("dve", lg, PS[:, 4, 0:NE], rb, ALU.add, ["pb4", X + "rb"], [X + "lg"])
            p.op("dve", lambda e: e.max(mx, lg), [X + "lg"], [X + "mx"])
            p.op("dve", lambda e: e.max_index(mxi, mx, lg), [X + "lg", X + "mx"], [X + "mxi"])
            p.cp("dve", idf, mxi, [X + "mxi"], [X + "idf"])
            p.ts("dve", nm0, mx[:, 0:1], -1.0, None, ALU.mult, None, [X + "mx"], [X + "nm0"])
            p.act(ex, mx[:, 0:4], AF.Exp, [X + "mx", X + "nm0"], [X + "ex", X + "esum"], bias=nm0, accum_out=esum)
            p.op("dve", lambda e: e.reciprocal(esum, esum), [X + "esum"], [X + "esum"])
            p.ts("dve", gates_all[:, i, :], ex, esum, None, ALU.mult, None, [X + "ex", X + "esum"], [X + "gates"])
            p.ts("dve", maskb, lg, mx[:, 3:4], None, ALU.is_ge, None, [X + "lg", X + "mx"], [X + "maskb"])
            p.mm(PS[:, 5, 0:NE], strib, maskb, True, True, [X + "maskb", X + "cb"], ["pb5"])
            p.mm(PS[:, 5, NE:2 * NE], onesb, maskb, True, True, [X + "maskb", X + "cb"], ["pb5"])
            for k in range(4):
                p.ts("dve", eq, iota32, idf[:, k:k + 1], None, ALU.is_equal, None, [X + "iota", X + "idf"], [X + "eq"])
                p.tt("dve", eq, eq, PS[:, 5, 0:NE], ALU.mult, [X + "eq", "pb5"], [X + "eq"])
                p.op("dve", lambda e, k=k: e.reduce_sum(posk[:, k:k + 1], eq, AX.X), [X + "eq"], [X + "posk"])
                p.ts("dve", eq, iota32, idf[:, k:k + 1], None, ALU.is_equal, None, [X + "iota", X + "idf", X + "posk"], [X + "eq"])
                p.tt("dve", eq, eq, cnt, ALU.mult, [X + "eq", X + "cnt"], [X + "eq"])
                p.op("dve", lambda e, k=k: e.reduce_sum(big[:, k:k + 1], eq, AX.X), [X + "eq"], [X + "big"])
            p.tt("dve", posk, posk, big, ALU.add, [X + "posk", X + "big"], [X + "posk"])
            p.tt("dve", cnt, cnt, PS[:, 5, NE:2 * NE], ALU.add, [X + "cnt", "pb5", X + "big"], [X + "cnt"])
            p.ts("dve", big, posk, float(CAP), 1.0e6, ALU.is_ge, ALU.mult, [X + "posk"], [X + "big"])
            p.ts("dve", slf, idf[:, 0:4], float(CAP), None, ALU.mult, None, [X + "idf"], [X + "slf"])
            p.tt("dve", slf, slf, posk, ALU.add, [X + "slf", X + "posk"], [X + "slf"])
            p.tt("dve", slf, slf, big, ALU.add, [X + "slf", X + "big"], [X + "slf"])
            p.ts("dve", slf, slf, float(NSLOT), None, ALU.min, None, [X + "slf"], [X + "slf"])
            p.cp("dve", slots_all[:, i, :], slf, [X + "slf"], [X + "slots"])
            for k in range(4):
                p.dma("pool", lambda e, xt=xt, i=i, k=k: e.indirect_dma_start(
                    out=XS, out_offset=bass.IndirectOffsetOnAxis(ap=slots_all[:, i, k:k + 1], axis=0),
                    in_=xt, in_offset=None, oob_is_err=False),
                    reads=[xtok, X + "slots", "xsz"], writes=["xs_%d_%d" % (i, k)])
        p.release(mP)
        if l == 0:
            dump("hmid", HBUF, ["hbuf%d" % i for i in range(int(os.environ.get("NTP7", NT)))], q="sp")
            dump("slots", slots_all.bitcast(F32), [X + "slots"], q="sp")
            dump("gates", gates_all, [X + "gates"], q="sp")
        if stop == "mix":
            break
        p.release(layB)

        m8 = p.mark()
        bgu = p.tile([NE * 16])
        p.dma("sp", lambda e: e.dma_start(out=bgu, in_=BGU[l]), writes=[X + "bgu"])
        gwr = [p.tile([8, 256], BF16) for _ in range(3)]
        dwr = [p.tile([8, D], BF16) for _ in range(2)]
        xer = [p.tile([NCT, D], F32R) for _ in range(2)]
        xeTr = [p.tile([8, CAP], BF16) for _ in range(2)]
        actTr = [p.tile([8, CAP], BF16) for _ in range(2)]
        glr = [p.tile([CAP]) for _ in range(2)]
        sgr = [p.tile([CAP]) for _ in range(2)]
        lnr = [p.tile([CAP]) for _ in range(2)]
        ytr = [p.tile([D]) for _ in range(2)]
        bdr = [p.tile([D]) for _ in range(2)]
        xs_toks = ["xs_%d_%d" % (i, k) for i in range(NT) for k in range(4)]
        npiece = 0
        nyt = 0
        for e_ in range(int(os.environ.get('NESIM', NE))):
            xe, xeT, actT, dw, bdb = xer[e_ % 2], xeTr[e_ % 2], actTr[e_ % 2], dwr[e_ % 2], bdr[e_ % 2]
            xetok, xeTtok, acttok, dwtok, bdtok = X + "xe%d" % (e_ % 2), X + "xeT%d" % (e_ % 2), X + "act%d" % (e_ % 2), X + "dw%d" % (e_ % 2), X + "bd%d" % (e_ % 2)
            p.dma("pool", lambda e, xe=xe, e_=e_: e.dma_start(out=xe, in_=XS[e_ * CAP:(e_ + 1) * CAP, :].rearrange("(j p) d -> p j d", p=128)),
                  reads=xs_toks + ["xsz"], writes=[xetok])
            p.dma("pool", lambda e, dw=dw, e_=e_: e.dma_start(out=dw, in_=WD[l, e_].rearrange("(j p) d -> p j d", p=128)), writes=[dwtok])
            p.dma("sp", lambda e, bdb=bdb, e_=e_: e.dma_start(out=bdb, in_=BD[l, e_:e_ + 1, :].partition_broadcast(128)), writes=[bdtok])
            xef = xe.bitcast(F32)
            for k in range(8):
                b = 6 + k % 2
                for jt in range(NCT):
                    p.tr(PS[:, b, jt * 128:(jt + 1) * 128], xef[:, jt, k * 128:(k + 1) * 128], ident, [xetok, "const"], ["pb%d" % b], inc=(jt == NCT - 1))
                if k % 2 == 0:
                    p.act(xeT[:, k, :], PS[:, b, 0:CAP], AF.Copy, ["pb%d" % b, "const"], [xeTtok], scale=pl[:, 8 + k:9 + k])
                else:
                    p.ts("dve", xeT[:, k, :], PS[:, b, 0:CAP], pl[:, 8 + k:9 + k], None, ALU.mult, None, ["pb%d" % b, "const"], [xeTtok])
            MD = int(os.environ.get('MOEDBG', '9'))
            for j in (range(8) if MD >= 2 else []):
                gw = gwr[npiece % 3]
                gwtok = X + "gw%d" % (npiece % 3)
                npiece += 1
                p.dma("pool", lambda e, gw=gw, e_=e_, j=j: e.dma_start(out=gw, in_=GU[l, e_, j].rearrange("p (k c) -> p k c", c=256), max_dma_last_dim=1024), writes=[gwtok])
                bg, bl = 2 * (j % 2), 2 * (j % 2) + 1
                for k in range(8):
                    p.mm(PS[:, bg, 0:CAP], gw[:, k, 0:128], xeT[:, k, :], k == 0, k == 7, [gwtok, xeTtok], ["pb%d" % bg], inc=(k == 7))
                for k in range(8):
                    p.mm(PS[:, bl, 0:CAP], gw[:, k, 128:256], xeT[:, k, :], k == 0, k == 7, [gwtok, xeTtok], ["pb%d" % bl], inc=(k == 7))
                gl, sg, ln = glr[j % 2], sgr[j % 2], lnr[j % 2]
                gt_, st_, lt_ = X + "gl%d" % (j % 2), X + "sg%d" % (j % 2), X + "ln%d" % (j % 2)
                bc = e_ * 16 + j * 2
                p.ts("dve", gl, PS[:, bg, 0:CAP], bgu[:, bc:bc + 1], 7.0, ALU.add, ALU.min, ["pb%d" % bg, X + "bgu"], [gt_])
                p.act(sg, gl, AF.Sigmoid, [gt_], [st_], scale=1.702)
                p.ts("dve", ln, PS[:, bl, 0:CAP], bgu[:, bc + 1:bc + 2], 7.0, ALU.add, ALU.min, ["pb%d" % bl, X + "bgu"], [lt_])
                p.ts("dve", ln, ln, -7.0, 1.0, ALU.max, ALU.add, [lt_], [lt_])
                p.tt("dve", gl, gl, sg, ALU.mult, [gt_, st_], [gt_])
                p.tt("dve", actT[:, j, :], gl, ln, ALU.mult, [gt_, lt_], [acttok])
            for jt in (range(NCT) if MD >= 3 else []):
                yt = ytr[nyt % 2]
                yttok = X + "yt%d" % (nyt % 2)
                nyt += 1
                for half in range(2):
                    b = 4 + half
                    for j in range(8):
                        p.mm(PS[:, b, :], actT[:, j, jt * 128:(jt + 1) * 128], dw[:, j, half * 512:(half + 1) * 512], j == 0, j == 7,
                             [acttok, dwtok], ["pb%d" % b], inc=(j == 7))
                    p.tt("dve", yt[:, half * 512:(half + 1) * 512], PS[:, b, :], bdb[:, half * 512:(half + 1) * 512], ALU.add,
                         ["pb%d" % b, bdtok], [yttok])
                r0 = e_ * CAP + jt * 128
                p.dma("sp", lambda e, yt=yt, r0=r0: e.dma_start(out=YS[r0:r0 + 128, :], in_=yt), reads=[yttok, xetok], writes=["ys_%d_%d" % (e_, jt)])
        p.release(m8)
        ys_toks = ["ys_%d_%d" % (e_, jt) for e_ in range(int(os.environ.get('NESIM', NE))) for jt in range(NCT)] if int(os.environ.get('MOEDBG', '9')) >= 3 else []
        last = (l == n_layers - 1) and stop is None
        hcr = [p.tile([D]) for _ in range(2)]
        ykr = [p.tile([D]) for _ in range(4)]
        if last:
            fng = p.tile([D])
            p.dma("sp", lambda e: e.dma_start(out=fng, in_=FNG.partition_broadcast(128)), writes=[X + "fng"])
            junk = p.tile([D])
            ss2 = p.tile([NT]); rs2 = p.tile([NT])
        for i in (range(int(os.environ.get("NTP7", NT))) if int(os.environ.get('MOEDBG', '9')) >= 4 else []):
            rows = slice(i * 128, (i + 1) * 128)
            hc = hcr[i % 2]
            hctok = X + "hc%d" % (i % 2)
            p.dma("sp", lambda e, hc=hc, rows=rows: e.dma_start(out=hc, in_=HBUF[rows, :]), reads=["hbuf%d" % i], writes=[hctok])
            for k in range(4):
                yk = ykr[k]
                yktok = X + "yk%d" % k
                p.dma("pool", lambda e, yk=yk, i=i, k=k: e.indirect_dma_start(
                    out=yk, out_offset=None, in_=YS, in_offset=bass.IndirectOffsetOnAxis(ap=slots_all[:, i, k:k + 1], axis=0),
                    oob_is_err=False), reads=ys_toks + [X + "slots", "xsz"], writes=[yktok])
                p.stt("dve", hc, yk, gates_all[:, i, k:k + 1], hc, ALU.mult, ALU.add, [yktok, hctok, X + "gates"], [hctok])
            if not last:
                p.dma("sp", lambda e, hc=hc, rows=rows: e.dma_start(out=HBUF[rows, :], in_=hc), reads=[hctok], writes=["hbuf%d" % i])
            else:
                p.act(junk, hc, AF.Square, [hctok], [X + "junk", X + "s2%d" % i], accum_out=ss2[:, i:i + 1])
                p.act(rs2[:, i:i + 1], ss2[:, i:i + 1], AF.Sqrt, [X + "s2%d" % i], [X + "r2%d" % i], bias=EPS, scale=1.0 / D)
                p.op("dve", lambda e, i=i: e.reciprocal(rs2[:, i:i + 1], rs2[:, i:i + 1]), [X + "r2%d" % i], [X + "r2%d" % i])
                p.stt("dve", hc, hc, rs2[:, i:i + 1], fng, ALU.mult, ALU.mult, [hctok, X + "r2%d" % i, X + "fng"], [hctok])
                tok = "out%d" % i
                p.dma("sp", lambda e, hc=hc, rows=rows: e.dma_start(out=OUT[rows, :], in_=hc), reads=[hctok], writes=[tok])
                final_tokens.append(tok)
        if l == 0:
            dump("hout", HBUF, ["hbuf%d" % i for i in range(int(os.environ.get("NTP7", NT)))], q="sp")
        p.release(m0)
        src_h = HBUF
        if stop == "moe":
            break

        p.release(lay0)

    if not dbg:
        pass
    p.finish(final_tokens)
    return nc


def kernel(**inputs):
    inp = {k: np.asarray(v) for k, v in inputs.items()}
    w = host_prep(inp)
    nc = build()
    x = np.ascontiguousarray(inp["x"], dtype=np.float32)
    nb = x.shape[0]
    in_maps = []
    for b in range(nb):
        m = dict(w)
        m["x"] = x[b]
        in_maps.append(m)
    res = run_bass_kernel_spmd(nc, in_maps, core_ids=list(range(nb)))
    return np.stack([np.asarray(r["out"]) for r in res.results], axis=0).astype(np.float32)
```

```python
import os
import types
import numpy as np
from contextlib import ExitStack
import concourse.bass as bass
import concourse.mybir as mybir
from concourse.bass_utils import run_bass_kernel_spmd

F32 = mybir.dt.float32
F32R = mybir.dt.float32r
BF16 = mybir.dt.bfloat16
I32 = mybir.dt.int32
U32 = mybir.dt.uint32
AF = mybir.ActivationFunctionType
ALU = mybir.AluOpType
AX = mybir.AxisListType

T = 2048
D = 1024
NT = 16
DEPTH = 2
DIN = 3332
NE = 32
CAP = 512
NCT = CAP // 128
EPS = 1e-6
NEG = -30000.0
NPP = 80

ENGS = ["pe", "act", "dve", "pool", "sp"]
N_DMA_SEMS = 40
ARENA_WORDS = 30208
ARENA_R_WORDS = 22528


def freeze(fn):
    if fn.__closure__ is None:
        return fn
    cells = []
    for c in fn.__closure__:
        try:
            cells.append(types.CellType(c.cell_contents))
        except ValueError:
            cells.append(c)
    return types.FunctionType(fn.__code__, fn.__globals__, fn.__name__, fn.__defaults__, tuple(cells))


class Prog:
    def __init__(self, nc):
        self.nc = nc
        self.es = ExitStack()
        self.ops = {e: [] for e in ENGS}
        self.cnt = {e: 0 for e in ENGS}
        self.pending = {e: False for e in ENGS}
        self.known = {e: {} for e in ENGS}
        self.last_w = {}
        self.readers = {}
        self.esem = {e: self.es.enter_context(nc.semaphore("sem_" + e)) for e in ENGS}
        self.dsem = [self.es.enter_context(nc.semaphore("dsem%d" % i)) for i in range(N_DMA_SEMS)]
        self.dval = [0] * N_DMA_SEMS
        self.isem = [self.es.enter_context(nc.semaphore("isem%d" % i)) for i in range(12)]
        self.iused = [False] * 12
        self.dnext = 0
        self.n_inst = 0
        self.arena = self.es.enter_context(nc.sbuf_tensor("arena", [128, ARENA_WORDS], F32))
        self.arena_r = self.es.enter_context(nc.sbuf_tensor("arena_r", [128, ARENA_R_WORDS], F32R))
        self.top_r = 0
        self.psum = self.es.enter_context(nc.psum_tensor("psum", [128, 8, 512], F32))
        self.top = 0
        self.uid = 0

    def alloc(self, words):
        off = self.top
        self.top += words
        assert self.top <= ARENA_WORDS, (self.top, ARENA_WORDS)
        return self.arena[:, off:off + words]

    def tile(self, shape, dt=F32):
        n = int(np.prod(shape))
        words = n if dt in (F32, F32R, I32, U32) else (n + 1) // 2
        if dt == F32R:
            off = self.top_r
            self.top_r += words
            assert self.top_r <= ARENA_R_WORDS, (self.top_r, ARENA_R_WORDS)
            ap = self.arena_r[:, off:off + words]
        else:
            ap = self.alloc(words)
            if dt != F32:
                ap = ap.bitcast(dt)
        if len(shape) == 2:
            ap = ap.rearrange("p (a b) -> p a b", b=shape[1])
        elif len(shape) == 3:
            ap = ap.rearrange("p (a b c) -> p a b c", b=shape[1], c=shape[2])
        return ap

    def name(self, base):
        self.uid += 1
        return "%s#%d" % (base, self.uid)

    def mark(self):
        return (self.top, self.top_r)

    def release(self, m):
        self.barrier()
        self.top, self.top_r = m

    def _need(self, eng, prod, waits):
        if prod is None:
            return
        if prod[0] == "e":
            _, e, idx = prod
            if e == eng and eng == "pe":
                return
            key = ("e", e)
        elif prod[0] == "i":
            waits[("i", prod[1], prod[2])] = 16
            return
        else:
            _, s, idx = prod
            key = ("d", s)
        if self.known[eng].get(key, 0) >= idx:
            return
        if idx > waits.get(key, 0):
            waits[key] = idx

    def _deps(self, eng, reads, writes):
        waits = {}
        for t in reads:
            self._need(eng, self.last_w.get(t), waits)
        for t in writes:
            self._need(eng, self.last_w.get(t), waits)
            for r in self.readers.get(t, ()):
                self._need(eng, r, waits)
        for key, idx in waits.items():
            if key[0] == "i":
                self.ops[eng].append(("wait", self.isem[key[1]], 16))
                continue
            self.known[eng][key] = idx
            sem = self.esem[key[1]] if key[0] == "e" else self.dsem[key[1]]
            self.ops[eng].append(("wait", sem, idx))

    def _mark(self, me, reads, writes):
        for t in reads:
            lst = self.readers.setdefault(t, [])
            lst[:] = [r for r in lst if not (r[0] == me[0] and r[1] == me[1])]
            lst.append(me)
        for t in writes:
            self.last_w[t] = me
            self.readers[t] = []

    def op(self, eng, fn, reads=(), writes=(), inc=True):
        fn = freeze(fn)
        self._deps(eng, reads, writes)
        if inc:
            self.cnt[eng] += 1
            idx = self.cnt[eng]
            self.pending[eng] = False
        else:
            idx = self.cnt[eng] + 1
            self.pending[eng] = True
        self.ops[eng].append(("inst", fn, inc))
        self._mark(("e", eng, idx), reads, writes)
        self.n_inst += 1

    def dma(self, q, fn, reads=(), writes=()):
        fn = freeze(fn)
        if q == "pool" and os.environ.get("SIMSEM"):
            sem = self.es.enter_context(self.nc.semaphore(self.name("fsem")))
            self.dsem.append(sem)
            self.dval.append(0)
            s = len(self.dsem) - 1
            self._deps(q, reads, writes)
            self.dval[s] = 16
            self.ops[q].append(("dma", fn, sem))
            self._mark(("d", s, 16), reads, writes)
            return
        s = self.dnext
        self.dnext = (self.dnext + 1) % N_DMA_SEMS
        if self.dval[s] > 0 and self.known[q].get(("d", s), 0) < self.dval[s]:
            self.known[q][("d", s)] = self.dval[s]
            self.ops[q].append(("wait", self.dsem[s], self.dval[s]))
        self._deps(q, reads, writes)
        self.dval[s] += 16
        self.ops[q].append(("dma", fn, self.dsem[s]))
        self._mark(("d", s, self.dval[s]), reads, writes)
        self.n_inst += 1

    def idma(self, slot, fn, reads=(), writes=(), wait_prev=False):
        q = "pool"
        if self.iused[slot]:
            if wait_prev:
                self.ops[q].append(("wait", self.isem[slot], 16))
            self.ops[q].append(("clear", self.isem[slot]))
        self._deps(q, reads, writes)
        self.iused[slot] = True
        self.uid += 1
        self.ops[q].append(("dma", fn, self.isem[slot]))
        self._mark(("i", slot, self.uid), reads, writes)
        self.n_inst += 1

    def isettle(self, slots, tokens, scratch):
        for sl in slots:
            if self.iused[sl]:
                self.ops["pool"].append(("wait", self.isem[sl], 16))
                self.ops["pool"].append(("clear", self.isem[sl]))
                self.iused[sl] = False
        self.cnt["pool"] += 1
        self.pending["pool"] = False
        self.ops["pool"].append(("inst", lambda e: e.memset(scratch, 0.0), True))
        me = ("e", "pool", self.cnt["pool"])
        for t in tokens:
            self.last_w[t] = me
            self.readers[t] = []

    def barrier(self):
        for f in ENGS:
            for e in ENGS:
                if e == f:
                    continue
                assert not self.pending[e]
                v = self.cnt[e]
                if v > self.known[f].get(("e", e), 0):
                    self.known[f][("e", e)] = v
                    self.ops[f].append(("wait", self.esem[e], v))
            for s in range(len(self.dsem)):
                v = self.dval[s]
                if v > self.known[f].get(("d", s), 0):
                    self.known[f][("d", s)] = v
                    self.ops[f].append(("wait", self.dsem[s], v))

    def finish(self, final_tokens):
        self._deps("sp", list(final_tokens), [])
        for e in ENGS:
            assert not self.pending[e], e
        nc = self.nc
        engmap = {"pe": "tensor", "act": "scalar", "dve": "vector", "pool": "gpsimd", "sp": "sync"}
        with nc.Block() as block:
            for e in ENGS:
                ops = self.ops[e]
                sem = self.esem[e]

                def body(engine, ops=ops, sem=sem):
                    for o in ops:
                        if o[0] == "wait":
                            engine.wait_ge(o[1], o[2])
                        elif o[0] == "clear":
                            engine.sem_clear(o[1])
                        elif o[0] == "inst":
                            ins = o[1](engine)
                            if o[2]:
                                ins.then_inc(sem, 1)
                        else:
                            o[1](engine).then_inc(o[2], 16)

                getattr(block, engmap[e])(body)
        self.es.close()

    def mm(self, out, lhsT, rhs, start, stop, reads, writes, inc=True):
        self.op("pe", lambda e: e.matmul(out, lhsT, rhs, start=start, stop=stop, skip_group_check=True), reads, writes, inc)

    def tr(self, out, in_, ident, reads, writes, inc=True):
        self.op("pe", lambda e: e.transpose(out, in_, ident), reads, writes, inc)

    def act(self, out, in_, func, reads, writes, bias=None, scale=None, accum_out=None):
        kw = {}
        if bias is not None:
            kw["bias"] = bias
        if scale is not None:
            kw["scale"] = scale
        if accum_out is not None:
            kw["accum_out"] = accum_out
        self.op("act", lambda e: e.activation(out, in_, func, **kw), reads, writes)

    def tt(self, eng, out, in0, in1, op, reads, writes):
        self.op(eng, lambda e: e.tensor_tensor(out, in0, in1, op), reads, writes)

    def ts(self, eng, out, in0, s1, s2, op0, op1, reads, writes):
        if s2 is None:
            self.op(eng, lambda e: e.tensor_scalar(out, in0, s1, None, op0), reads, writes)
        else:
            self.op(eng, lambda e: e.tensor_scalar(out, in0, s1, s2, op0, op1), reads, writes)

    def stt(self, eng, out, in0, scalar, in1, op0, op1, reads, writes):
        self.op(eng, lambda e: e.scalar_tensor_tensor(out, in0, scalar, in1, op0, op1), reads, writes)

    def cp(self, eng, out, in_, reads, writes):
        if eng == "act":
            self.op("act", lambda e: e.copy(out, in_), reads, writes)
        else:
            self.op(eng, lambda e: e.tensor_copy(out, in_), reads, writes)

    def memset(self, eng, out, val, writes):
        self.op(eng, lambda e: e.memset(out, val), (), writes)


def host_consts():
    c = {}
    c["ident"] = np.eye(128, dtype=np.float32)
    j = np.arange(128)[:, None]
    s = np.arange(128)[None, :]
    c["trim"] = (-1.0 * (j >= s)).astype(np.float32)
    c["ones"] = np.ones((128, 128), np.float32)
    c["bones"] = ((j // 64) == (s // 64)).astype(np.float32)
    c["maskle"] = (j <= s).astype(np.float32)
    c["negle"] = (NEG * (j > s)).astype(np.float32)
    e = np.zeros((128, 128), np.float32)
    e[127, :] = 1.0
    c["e127"] = e
    m64 = np.zeros((128, 2, 64), np.float32)
    ss_ = np.arange(128)[:, None]
    tt_ = np.arange(64)[None, :]
    m64[:, 0, :] = (ss_ < 64) & (ss_ <= tt_)
    m64[:, 1, :] = (ss_ >= 64) & (ss_ - 64 <= tt_)
    c["mask64"] = m64
    c["stri"] = (j < s).astype(np.float32)
    c["zeros"] = np.zeros((1024, D), np.float32)
    c["iota32"] = np.tile(np.arange(32, dtype=np.float32)[None, :], (128, 1))
    nm = np.zeros((128, 4, 512), np.float32)
    for jb in range(4):
        ss = jb * 128 + np.arange(128)[:, None]
        tt = np.arange(512)[None, :]
        nm[:, jb, :] = NEG * (ss >= tt)
    c["negmask"] = nm
    selh = np.zeros((36, 4, 128), np.float32)
    for h in range(4):
        selh[32 + h, h, :] = 1.0
    c["selh"] = selh
    selg = np.zeros((36, 2, 128), np.float32)
    for g in range(2):
        for hh in range(2):
            selg[32 + 2 * g + hh, g, hh * 64:(hh + 1) * 64] = 1.0
    c["selg"] = selg
    return c


def pk(v):
    return np.ascontiguousarray(v.reshape(-1, 128).T)


def host_prep(inp, moe=True):
    w = {}
    pp = np.zeros((DEPTH, 128, NPP), np.float32)
    for l in range(DEPTH):
        pp[l, :, 0:8] = pk(inp["norm_mix_g"][l])
        pp[l, :, 8:16] = pk(inp["norm_ffn_g"][l])
        pp[l, :, 16:18] = pk(inp["sb_norm_g"][l])
        pp[l, :, 18:20] = pk(inp["rg_norm_g"][l])
        pp[l, :, 20:22] = pk(inp["hg_norm_g"][l])
        pp[l, :, 22:24] = pk(inp["m2_norm_g"][l])
        for k in range(4):
            pp[l, :, 24 + k:24 + 8:4] = pk(inp["rg_conv_w"][l, k])
        pp[l, :, 32:34] = pk(inp["rg_conv_b"][l])
        pp[l, :, 34:36] = pk(inp["rg_ba"][l])
        pp[l, :, 36:38] = pk(inp["rg_bx"][l])
        pp[l, :, 38:40] = pk(inp["rg_lambda"][l])
        pp[l, :, 40:42] = pk(inp["hg_lower_bounds"][0])
        pp[l, :, 42:44] = pk(inp["hg_lower_bounds"][1])
        for k in range(4):
            pp[l, :, 44 + k:44 + 24:4] = pk(inp["m2_conv_w"][l, k])
        pp[l, :, 68:74] = pk(inp["m2_conv_b"][l])
        for g in range(2):
            pp[l, 0:64, 74 + g] = inp["m2_d"][l, 2 * g]
            pp[l, 64:128, 74 + g] = inp["m2_d"][l, 2 * g + 1]
        pp[l, 0:4, 76] = inp["m2_dt_bias"][l]
        pp[l, 32:36, 76] = inp["m2_dt_bias"][l]
        pp[l, 0:4, 77] = inp["m2_a_log"][l]
        pp[l, 32:36, 77] = inp["m2_a_log"][l]
    w["pp"] = pp
    w["w_in"] = np.ascontiguousarray(inp["w_in"])
    wdt = np.zeros((DEPTH, D, 36), np.float32)
    wdt[:, :, 0:4] = inp["w_in"][:, :, 3328:3332]
    wdt[:, :, 32:36] = inp["w_in"][:, :, 3328:3332]
    w["w_dt2"] = wdt
    w["w_out"] = np.ascontiguousarray(inp["w_out"])
    rgw = np.zeros((DEPTH, 2, 2, 128, 128), np.float32)
    for l in range(DEPTH):
        for gi, nm in enumerate(["rg_wa", "rg_wx"]):
            for c in range(2):
                for hh in range(2):
                    rgw[l, gi, c, hh * 64:(hh + 1) * 64, hh * 64:(hh + 1) * 64] = inp[nm][l, 2 * c + hh]
    w["rgw"] = rgw
    w["router_w"] = np.ascontiguousarray(inp["router_w"])
    w["router_b"] = np.ascontiguousarray(inp["router_b"])
    w["fng"] = np.ascontiguousarray(inp["final_norm_g"]).reshape(1, D)
    w.update(host_consts())
    if not moe:
        return w
    gu = inp["moe_w_gu"].reshape(DEPTH, NE, 8, 128, 8, 128, 2)
    gu = gu.transpose(0, 1, 4, 3, 2, 6, 5)
    w["gu"] = np.ascontiguousarray(gu).reshape(DEPTH, NE, 8, 128, 8 * 256)
    w["wd"] = np.ascontiguousarray(inp["moe_w_down"])
    bgu = inp["moe_b_gu"].reshape(DEPTH, NE, 8, 128, 2).transpose(0, 3, 1, 2, 4)
    w["bgu"] = np.ascontiguousarray(bgu).reshape(DEPTH, 128, NE * 16)
    w["bd"] = np.ascontiguousarray(inp["moe_b_down"])
    return w


def build(dbg=None, n_layers=DEPTH, stop=None):
    nc = bass.Bass("TRN2", target_bir_lowering=False)

    def din(name, shape, dt=F32):
        return nc.dram_tensor(name, list(shape), dt, kind="ExternalInput").ap()

    X = din("x", [T, D])
    PP = din("pp", [DEPTH, 128, NPP])
    W_IN = din("w_in", [DEPTH, D, DIN])
    W_DT2 = din("w_dt2", [DEPTH, D, 36])
    W_OUT = din("w_out", [DEPTH, D, D])
    RGW = din("rgw", [DEPTH, 2, 2, 128, 128])
    ROUTER_W = din("router_w", [DEPTH, D, NE])
    ROUTER_B = din("router_b", [DEPTH, NE])
    if stop in (None, "moe"):
        GU = din("gu", [DEPTH, NE, 8, 128, 2048])
        WD = din("wd", [DEPTH, NE, D, D])
        BGU = din("bgu", [DEPTH, 128, NE * 16])
        BD = din("bd", [DEPTH, NE, D])
    FNG = din("fng", [1, D])
    CONST = {k: din(k, v.shape) for k, v in host_consts().items()}
    OUT = nc.dram_tensor("out", [T, D], F32, kind="ExternalOutput").ap()
    HBUF = nc.dram_tensor("hbuf", [T, D], F32, kind="Internal").ap()
    NSLOT = NE * CAP
    XS = nc.dram_tensor("xs_buf", [NSLOT + 128, D], F32, kind="Internal").ap()
    YS = XS
    dbg_out = {}
    if dbg:
        for k, shp in dbg.items():
            dbg_out[k] = nc.dram_tensor("dbg_" + k, list(shp), F32, kind="ExternalOutput").ap()

    p = Prog(nc)
    PS = p.psum
    final_tokens = []

    def bank(i):
        return PS[:, i, :]

    ident = p.tile([128])
    identr = p.tile([128], F32R)
    trim = p.tile([128], F32R)
    onesr = p.tile([128], F32R)
    bonesr = p.tile([128], F32R)
    bones = p.tile([128])
    maskle = p.tile([128])
    negle = p.tile([128])
    e127 = p.tile([128])
    ppt = p.tile([DEPTH, NPP])
    for ap, nm, q in [(ident, "ident", "sp"), (bones, "bones", "sp"), (maskle, "maskle", "sp"),
                      (negle, "negle", "sp"), (e127, "e127", "sp"),
                      (identr, "ident", "pool"), (trim, "trim", "pool"), (onesr, "ones", "pool"),
                      (bonesr, "bones", "pool")]:
        p.dma(q, lambda e, ap=ap, nm=nm: e.dma_start(out=ap, in_=CONST[nm]), writes=["const"])
    p.dma("sp", lambda e: e.dma_start(out=ppt, in_=PP.rearrange("l p n -> p l n")), writes=["const"])
    p.barrier()

    for zi in range(NSLOT // 1024):
        p.dma("sp", lambda e, zi=zi: e.dma_start(out=XS[zi * 1024:(zi + 1) * 1024, :], in_=CONST["zeros"]), writes=["xsz"])
    p.dma("sp", lambda e: e.dma_start(out=XS[NSLOT:NSLOT + 128, :], in_=CONST["zeros"][0:128, :]), writes=["xsz"])

    bregs = {}

    def bnd(e):
        if "r" not in bregs:
            r = e.alloc_register("bnd")
            e.reg_mov(r, NSLOT - 1)
            bregs["r"] = r
        return bregs["r"]

    def dump(name, src_ap, reads, q="pool"):
        if name in dbg_out:
            tok = p.name("dbg")
            p.dma(q, lambda e: e.dma_start(out=dbg_out[name], in_=src_ap), reads=reads, writes=[tok, "dbgw_" + name])
            final_tokens.append(tok)

    def rms_tokens(src_dram, l, gcol, on_tile):
        m0 = p.mark()
        hr = [p.tile([D]) for _ in range(2)]
        xh = [p.tile([D]) for _ in range(2)]
        ss = p.tile([NT])
        rstd = p.tile([NT])
        for i in range(NT):
            ht, xt = hr[i % 2], xh[i % 2]
            htok, xtok = "hr%d" % (i % 2), "xh%d" % (i % 2)
            p.dma("sp", lambda e, ht=ht, i=i: e.dma_start(out=ht, in_=src_dram[i * 128:(i + 1) * 128, :]), writes=[htok])
            p.act(xt, ht, AF.Square, [htok], [xtok, "ss%d" % i], accum_out=ss[:, i:i + 1])
            p.act(rstd[:, i:i + 1], ss[:, i:i + 1], AF.Sqrt, ["ss%d" % i], ["rs%d" % i], bias=EPS, scale=1.0 / D)
            p.op("dve", lambda e, i=i: e.reciprocal(rstd[:, i:i + 1], rstd[:, i:i + 1]), ["rs%d" % i], ["rs%d" % i])
            p.act(xt, ht, AF.Copy, [htok, "rs%d" % i], [xtok], scale=rstd[:, i:i + 1])
            on_tile(i, xt, xtok, rstd[:, i:i + 1], ht, htok)
        return m0

    def transpose_tile(i, xt, xtok, dst, dsttok, gcols, banks, dst_is_bf16=True):
        for half in range(2):
            b = banks[half]
            btok = "pb%d" % b
            for kk in range(4):
                k = half * 4 + kk
                p.tr(PS[:, b, kk * 128:(kk + 1) * 128], xt[:, k * 128:(k + 1) * 128], ident,
                     [xtok, "const"], [btok], inc=(kk == 3))
            src = PS[:, b, :].rearrange("p (a b) -> p a b", b=128)
            gb = gcols[:, half * 4:half * 4 + 4].rearrange("p (a b) -> p a b", b=1).to_broadcast([128, 4, 128])
            p.tt("dve", dst[:, half * 4:half * 4 + 4, i * 128:(i + 1) * 128], src, gb, ALU.mult,
                 [btok, "const"], [dsttok])

    wring = {"n": 0}

    def load_w(l, col0, ncols, src=None):
        wt = p.tile([8, ncols], BF16)
        tok = p.name("w")
        s = (W_IN[l] if src is None else src).rearrange("(k p) c -> p k c", p=128)[:, :, col0:col0 + ncols]
        p.dma("pool", lambda e: e.dma_start(out=wt, in_=s), writes=[tok])
        return wt, tok

    bankrr = {"n": 0}

    def proj_fm(uT, wt, wtok, ncols, evac, banks, cbs=None):
        ncb = (ncols + 127) // 128
        for cb in (range(ncb) if cbs is None else cbs):
            cw = min(128, ncols - cb * 128)
            for tg in range(4):
                b = banks[bankrr["n"] % len(banks)]
                bankrr["n"] += 1
                btok = "pb%d" % b
                for k in range(8):
                    p.mm(PS[0:cw, b, :], wt[:, k, cb * 128:cb * 128 + cw], uT[:, k, tg * 512:(tg + 1) * 512],
                         k == 0, k == 7, ["uT", wtok], [btok], inc=(k == 7))
                evac(cb, tg, b, btok)

    def proj_tm(uT, wt, wtok, ncols, evac, banks):
        for i in range(NT):
            b = banks[bankrr["n"] % len(banks)]
            bankrr["n"] += 1
            btok = "pb%d" % b
            for k in range(8):
                p.mm(PS[:, b, 0:ncols], uT[:, k, i * 128:(i + 1) * 128], wt[:, k, :],
                     k == 0, k == 7, ["uT", wtok], [btok], inc=(k == 7))
            evac(i, b, btok)

    evrr = {"n": 0}

    def evac_copy(out, in_, reads, writes):
        eng = "act" if evrr["n"] % 2 == 0 else "dve"
        evrr["n"] += 1
        p.cp(eng, out, in_, reads, writes)

    def fm_rmsnorm(src, srctok, nch, lhs, joint, gcols, dst_c0, mixT, extra=None, extratok=None, banks=(6, 7)):
        m0 = p.mark()
        sq = [p.tile([512], F32R) for _ in range(2)]
        rs = [p.tile([512]) for _ in range(2)]
        n = 0
        for tg in range(4):
            sl = slice(tg * 512, (tg + 1) * 512)
            groups = [list(range(nch))] if joint else [[c] for c in range(nch)]
            for grp in groups:
                b = banks[n % 2]
                btok = "pb%d" % b
                for ci, c in enumerate(grp):
                    sqt = sq[n % 2]
                    sqtok = "nsq%d" % (n % 2)
                    p.act(sqt, src[:, c, sl], AF.Square, [srctok], [sqtok])
                    p.mm(PS[:, b, :], lhs, sqt, ci == 0, ci == len(grp) - 1, [sqtok, "const"], [btok])
                    n += 1
                rt = rs[n % 2]
                rtok = "nrs%d" % (n % 2)
                denom = 128.0 * len(grp) if joint else (128.0 if lhs is onesr else 64.0)
                p.act(rt, PS[:, b, :], AF.Sqrt, [btok], [rtok], bias=EPS, scale=1.0 / denom)
                p.op("dve", lambda e, rt=rt: e.reciprocal(rt, rt), [rtok], [rtok])
                for c in grp:
                    if extra is None:
                        p.stt("dve", mixT[:, dst_c0 + c, sl], src[:, c, sl], gcols[:, c:c + 1], rt, ALU.mult, ALU.mult,
                              [srctok, rtok, "const"], ["mixT"])
                    else:
                        p.stt("dve", src[:, c, sl], src[:, c, sl], gcols[:, c:c + 1], rt, ALU.mult, ALU.mult,
                              [srctok, rtok, "const"], [srctok])
                        p.tt("dve", mixT[:, dst_c0 + c, sl], src[:, c, sl], extra[:, c, sl], ALU.mult,
                             [srctok, extratok], ["mixT"])
        p.release(m0)

    src_h = X
    for l in range(n_layers):
        pl = ppt[:, l, :]
        lay0 = p.mark()
        gates_all = p.tile([NT, 4])
        slots_all = p.tile([NT, 4], I32)
        layB = p.mark()
        uT = p.tile([8, T], BF16)
        mixT = p.tile([8, T], BF16)

        def on_tile_p1(i, xt, xtok, rstd, ht, htok):
            transpose_tile(i, xt, xtok, uT, "uT", pl[:, 0:8], (0, 1) if i % 2 == 0 else (2, 3))
        m0 = rms_tokens(src_h, l, 0, on_tile_p1)
        p.release(m0)
        if l == 0:
            dump("uT", uT, ["uT"])
        if stop == "p1":
            break

        SKIP = os.environ.get('SKIPAB', '')
        if 'A' not in SKIP:
            m0 = p.mark()
            negmask = p.tile([4, 512], F32R)
            p.dma("pool", lambda e: e.dma_start(out=negmask, in_=CONST["negmask"]), writes=["negmask"])
            qT = p.tile([2, T], F32R)
            kT = p.tile([2, T], F32R)
            vtm = p.tile([NT, 256], F32R)
            oaT = p.tile([2, T])
            wq, wqtok = load_w(l, 0, 512)
            wv, wvtok = load_w(l, 512, 256)

            def ev_qk(cb, tg, b, btok):
                sl = slice(tg * 512, (tg + 1) * 512)
                if cb < 2:
                    p.act(qT[:, cb, sl], PS[:, b, :], AF.Copy, [btok], ["qT"], scale=0.125)
                else:
                    p.cp("dve", kT[:, cb - 2, sl], PS[:, b, :], [btok], ["kT"])
            proj_fm(uT, wq, wqtok, 512, ev_qk, (0, 1, 2, 3))

            def ev_v(i, b, btok):
                evac_copy(vtm[:, i, :], PS[:, b, 0:256], [btok], ["vtm"])
            proj_tm(uT, wv, wvtok, 256, ev_v, (0, 1, 2, 3))

            m1 = p.mark()
            Er = [p.tile([2, 512]) for _ in range(2)]
            SPr = [p.tile([2, 512], F32R) for _ in range(2)]
            SSr = [p.tile([2, 512], F32R) for _ in range(2)]
            ATr = [p.tile([2, 512], F32R) for _ in range(3)]
            items = []
            for sb in range(4):
                for c in range(2):
                    nkb = 4 * sb + 4
                    for kb in range(nkb - 1, -1, -1):
                        items.append((c, sb, kb, kb == nkb - 1, kb == 0, kb >= 4 * sb))
            NI = len(items)

            def zbanks(n):
                return 2 * (n % 3)

            def s1(n):
                c, sb, kb, first, last, diag = items[n]
                zb = zbanks(n)
                for hh in range(2):
                    ps = slice(hh * 64, (hh + 1) * 64)
                    p.mm(PS[:, zb + hh, :], kT[ps, c, kb * 128:(kb + 1) * 128], qT[ps, c, sb * 512:(sb + 1) * 512],
                         True, not diag, ["kT", "qT"], ["pb%d" % (zb + hh)], inc=(not diag))
                    if diag:
                        p.mm(PS[:, zb + hh, :], identr, negmask[:, kb - 4 * sb, :], False, True,
                             ["const", "negmask"], ["pb%d" % (zb + hh)])

            def s2(n):
                c, sb, kb, first, last, diag = items[n]
                zb = zbanks(n)
                ztoks = ["pb%d" % zb, "pb%d" % (zb + 1)]
                z2 = PS[:, zb:zb + 2, :]
                p.act(Er[n % 2], z2, AF.Exp, ztoks, ["E%d" % (n % 2)])
                p.act(SPr[n % 2], Er[n % 2], AF.Ln, ["E%d" % (n % 2)], ["SP%d" % (n % 2)], bias=1.0)

            def s3(n):
                c, sb, kb, first, last, diag = items[n]
                zb = zbanks(n)
                for hh in range(2):
                    p.mm(PS[:, zb + hh, :], trim, SPr[n % 2][:, hh, :], False, first,
                         ["SP%d" % (n % 2), "const"], ["pb%d" % (zb + hh)], inc=first)
                    if not first:
                        p.mm(PS[:, zb + hh, :], onesr, SSr[(n - 1) % 2][:, hh, :], False, True,
                             ["SS%d" % ((n - 1) % 2), "const"], ["pb%d" % (zb + hh)])
                if not last:
                    if first:
                        p.ts("dve", SSr[n % 2], SPr[n % 2], -1.0, None, ALU.mult, None, ["SP%d" % (n % 2)], ["SS%d" % (n % 2)])
                    else:
                        p.tt("dve", SSr[n % 2], SSr[(n - 1) % 2], SPr[n % 2], ALU.subtract,
                             ["SS%d" % ((n - 1) % 2), "SP%d" % (n % 2)], ["SS%d" % (n % 2)])

            def s4(n):
                zb = zbanks(n)
                p.act(ATr[n % 3], PS[:, zb:zb + 2, :], AF.Exp, ["pb%d" % zb, "pb%d" % (zb + 1)], ["AT%d" % (n % 3)])

            def s5(n):
                c, sb, kb, first, last, diag = items[n]
                for hh in range(2):
                    p.mm(PS[:, 6 + hh, :], vtm[:, kb, c * 128:(c + 1) * 128], ATr[n % 3][:, hh, :], first, last,
                         ["vtm", "AT%d" % (n % 3)], ["pb%d" % (6 + hh)])
                if last:
                    for hh in range(2):
                        ps = slice(hh * 64, (hh + 1) * 64)
                        p.cp("dve", oaT[ps, c, sb * 512:(sb + 1) * 512], PS[ps, 6 + hh, :], ["pb%d" % (6 + hh)], ["oaT"])

            for n in range(NI + 2):
                if n < NI:
                    s1(n)
                    s2(n)
                if 1 <= n <= NI:
                    s3(n - 1)
                    s4(n - 1)
                if 2 <= n <= NI + 1:
                    s5(n - 2)
            p.release(m1)
            if l == 0:
                dump("oaT", oaT, ["oaT"])
            fm_rmsnorm(oaT, "oaT", 2, onesr, True, pl[:, 16:18], 0, mixT, banks=(0, 1))
            p.release(m0)
            if l == 0:
                dump("mixT", mixT, ["mixT"])
            if stop == "A":
                break

            m0 = p.mark()
            obT = p.tile([2, T])
            rgwt = p.tile([4, 128], F32R)
            p.dma("pool", lambda e: e.dma_start(out=rgwt, in_=RGW[l].rearrange("g c i j -> i (g c) j")), writes=["rgw"])
            nsl = p.tile([2])
            nsl2 = p.tile([2])
            p.act(nsl, pl[:, 38:40], AF.Exp, ["const"], ["nsl"], scale=-1.0)
            p.act(nsl, nsl, AF.Ln, ["nsl"], ["nsl"], bias=1.0)
            p.ts("dve", nsl, nsl, -8.0, None, ALU.mult, None, ["nsl"], ["nsl"])
            p.ts("dve", nsl2, nsl, 2.0, None, ALU.mult, None, ["nsl"], ["nsl2"])
            for c in range(2):
                m1 = p.mark()
                wx, wxtok = load_w(l, 768 + c * 128, 128)
                wg, wgtok = load_w(l, 1024 + c * 128, 128)
                xp = p.tile([T + 3])
                xcR = p.tile([T], F32R); xc = xcR.bitcast(F32)
                gtR = p.tile([T], F32R); gt = gtR.bitcast(F32)
                rR = p.tile([T], F32R); rr = rR.bitcast(F32)
                igR = p.tile([T], F32R); ig = igR.bitcast(F32)
                sR = p.tile([T], F32R); s_ = sR.bitcast(F32)
                at = p.tile([T])
                mt = p.tile([T])
                X = "B_"
                p.memset("pool", xp[:, 0:3], 0.0, [X + "xp"])

                def ev_x(cb, tg, b, btok, xp=xp):
                    evac_copy(xp[:, 3 + tg * 512:3 + (tg + 1) * 512], PS[:, b, :], [btok], [X + "xp"])
                proj_fm(uT, wx, wxtok, 128, ev_x, (0, 1, 2, 3))

                def ev_g(cb, tg, b, btok, gtR=gtR):
                    evac_copy(gtR[:, tg * 512:(tg + 1) * 512], PS[:, b, :], [btok], [X + "gt"])
                proj_fm(uT, wg, wgtok, 128, ev_g, (0, 1, 2, 3))
                cw = 24 + c * 4
                p.ts("dve", xcR, xp[:, 3:3 + T], pl[:, cw + 3:cw + 4], pl[:, 32 + c:33 + c], ALU.mult, ALU.add,
                     [X + "xp", "const"], [X + "xc"])
                for k in range(3):
                    p.stt("dve", xcR, xp[:, k:k + T], pl[:, cw + k:cw + k + 1], xc, ALU.mult, ALU.add,
                          [X + "xp", X + "xc", "const"], [X + "xc"])
                for tg in range(4):
                    sl = slice(tg * 512, (tg + 1) * 512)
                    for gi, (dst, bcol, tokn) in enumerate([(rR, 34, "r"), (igR, 36, "ig")]):
                        b = 4 + (2 * tg + gi) % 4
                        p.mm(PS[:, b, :], rgwt[:, gi * 2 + c, :], xcR[:, sl], True, True, [X + "xc", "rgw"], ["pb%d" % b])
                        p.act(dst[:, sl], PS[:, b, :], AF.Sigmoid, ["pb%d" % b, "const"], [X + tokn],
                              bias=pl[:, bcol + c:bcol + c + 1])
                p.act(at, rr, AF.Exp, [X + "r", "nsl"], [X + "a"], scale=nsl[:, c:c + 1])
                p.act(mt, rr, AF.Exp, [X + "r", "nsl2"], [X + "m"], scale=nsl2[:, c:c + 1])
                p.act(mt, mt, AF.Relu, [X + "m"], [X + "m"], scale=-1.0, bias=1.0)
                p.act(mt, mt, AF.Sqrt, [X + "m"], [X + "m"])
                p.memset("dve", mt[:, 0:1], 1.0, [X + "m"])
                p.tt("dve", mt, mt, ig, ALU.mult, [X + "m", X + "ig"], [X + "m"])
                p.tt("dve", mt, mt, xc, ALU.mult, [X + "m", X + "xc"], [X + "m"])
                p.op("dve", lambda e, c=c, at=at, mt=mt: e.tensor_tensor_scan(obT[:, c, :], at, mt, 0.0, ALU.mult, ALU.add),
                     [X + "a", X + "m"], ["obT"])
                p.act(sR, gt, AF.Square, [X + "gt"], [X + "s"])
                p.ts("dve", sR, s_, 0.044715, 1.0, ALU.mult, ALU.add, [X + "s"], [X + "s"])
                p.tt("dve", sR, s_, gt, ALU.mult, [X + "s", X + "gt"], [X + "s"])
                p.act(sR, s_, AF.Sigmoid, [X + "s"], [X + "s"], scale=1.5957691216)
                p.tt("dve", sR, s_, gt, ALU.mult, [X + "s", X + "gt"], [X + "s"])
                p.tt("dve", obT[:, c, :], obT[:, c, :], s_, ALU.mult, ["obT", X + "s"], ["obT"])
                p.release(m1)
            fm_rmsnorm(obT, "obT", 2, onesr, True, pl[:, 18:20], 2, mixT, banks=(0, 1))
            p.release(m0)
            if l == 0:
                dump("mixT", mixT, ["mixT"])
            if stop == "B":
                break

        if 'C' not in SKIP:
            m0 = p.mark()
            X = "C_"
            NC2 = 32
            lb = p.tile([2])
            oml = p.tile([2])
            ones128 = p.tile([128])
            mask64 = p.tile([2, 64])
            p.dma("sp", lambda e: e.dma_start(out=mask64, in_=CONST["mask64"]), writes=[X + "mask"])
            p.memset("pool", ones128, 1.0, [X + "ones"])
            if l == 0:
                p.memset("dve", lb, 0.0, [X + "lb"])
            else:
                p.tt("dve", lb, pl[:, 42:44], pl[:, 40:42], ALU.subtract, ["const"], [X + "lb"])
                p.act(lb, lb, AF.Sigmoid, [X + "lb"], [X + "lb"])
            p.ts("dve", oml, lb, -1.0, 1.0, ALU.mult, ALU.add, [X + "lb"], [X + "oml"])
            for c in range(2):
                m1 = p.mark()
                T1 = p.tile([T]); T2 = p.tile([T]); T3 = p.tile([T]); T4 = p.tile([T]); T5 = p.tile([T])
                T6R = p.tile([T], F32R); T7R = p.tile([T], F32R); T8R = p.tile([T], F32R)
                khatR = p.tile([NT, 128], F32R)
                vvzR = p.tile([NT, 2, 128], F32R)
                ST2R = p.tile([NC2 + 1, 128], F32R)
                ST2 = ST2R.bitcast(F32)
                scmR = [p.tile([2, 64], F32R) for _ in range(2)]
                dl = p.tile([NC2])
                utmp = [p.tile([128]) for _ in range(2)]
                v3 = lambda a: a.rearrange("p (n q) -> p n q", q=64)
                p.ts("dve", vvzR.rearrange("p a b c -> p (a b c)"), uT[:, 0:2, :].rearrange("p a b -> p (a b)"),
                     0.0, None, ALU.mult, None, ["uT"], [X + "vvz"])
                p.ts("pool", ST2R[:, 0, :], ones128, 0.0, None, ALU.mult, None, [X + "ones"], [X + "st0"])
                mw = p.mark()
                wq_, wqt = load_w(l, 1280 + c * 128, 128)
                wf_, wft = load_w(l, 1536 + c * 128, 128)
                wi_, wit = load_w(l, 1792 + c * 128, 128)

                def ev_q(cb, tg, b, btok, T1=T1):
                    p.act(T1[:, tg * 512:(tg + 1) * 512], PS[:, b, :], AF.Silu, [btok], [X + "T1"])
                proj_fm(uT, wq_, wqt, 128, ev_q, (0, 1, 2, 3))

                def ev_f(cb, tg, b, btok, T2=T2):
                    p.act(T2[:, tg * 512:(tg + 1) * 512], PS[:, b, :], AF.Sigmoid, [btok], [X + "T2"])
                proj_fm(uT, wf_, wft, 128, ev_f, (0, 1, 2, 3))

                def ev_i(i, b, btok, vvzR=vvzR):
                    p.cp("dve", vvzR[:, i, 0, 0:64], PS[:, b, 0:64], [btok], [X + "vvz"])
                    p.cp("dve", vvzR[:, i, 1, 64:128], PS[:, b, 64:128], [btok], [X + "vvz"])
                proj_tm(uT, wi_, wit, 128, ev_i, (4, 5, 6, 7))
                p.release(mw)
                p.ts("dve", T2, T2, oml[:, c:c + 1], lb[:, c:c + 1], ALU.mult, ALU.add, [X + "T2", X + "oml", X + "lb"], [X + "T2"])
                p.act(T3, T2, AF.Ln, [X + "T2"], [X + "T3"])
                p.ts("dve", T2, T2, -1.0, 1.0, ALU.mult, ALU.add, [X + "T2"], [X + "T2"])
                for n in range(NC2):
                    sl = slice(n * 64, (n + 1) * 64)
                    p.op("dve", lambda e, sl=sl, T4=T4, T3=T3: e.tensor_tensor_scan(T4[:, sl], ones128[:, 0:64], T3[:, sl], 0.0, ALU.mult, ALU.add),
                         [X + "T3", X + "ones"], [X + "T4"])
                b3 = v3(T4)
                p.tt("dve", v3(T3), b3, b3[:, :, 31:32].to_broadcast([128, NC2, 64]), ALU.subtract, [X + "T4"], [X + "T3"])
                p.ts("dve", T5, T3, 40.0, None, ALU.min, None, [X + "T3"], [X + "T5"])
                p.act(T5, T5, AF.Exp, [X + "T5"], [X + "T5"])
                p.tt("dve", T6R, T1, T5, ALU.mult, [X + "T1", X + "T5"], [X + "T6"])
                p.ts("dve", T5, T3, -40.0, None, ALU.max, None, [X + "T3"], [X + "T5"])
                p.act(T5, T5, AF.Exp, [X + "T5"], [X + "T5"], scale=-1.0)
                p.tt("dve", T7R, T2, T5, ALU.mult, [X + "T2", X + "T5"], [X + "T7"])
                p.act(T5, T4, AF.Exp, [X + "T4"], [X + "T5"])
                p.tt("dve", T8R, T1, T5, ALU.mult, [X + "T1", X + "T5"], [X + "T8"])
                p.tt("dve", v3(T3), b3[:, :, 63:64].to_broadcast([128, NC2, 64]), b3, ALU.subtract, [X + "T4"], [X + "T3"])
                p.act(T5, T3, AF.Exp, [X + "T3"], [X + "T5"])
                p.tt("dve", T3, T2, T5, ALU.mult, [X + "T2", X + "T5"], [X + "T3"])
                p.act(dl, b3[:, :, 63], AF.Exp, [X + "T4"], [X + "dl"])
                for n4 in range(4):
                    b = 4 + n4 % 2
                    for j in range(4):
                        n = n4 * 4 + j
                        p.tr(PS[:, b, j * 128:(j + 1) * 128], T3[:, n * 128:(n + 1) * 128], ident, [X + "T3", "const"], ["pb%d" % b], inc=(j == 3))
                    p.cp("act", khatR[:, n4 * 4:n4 * 4 + 4, :], PS[:, b, :].rearrange("p (a b) -> p a b", b=128), ["pb%d" % b], [X + "khat"])
                CS = int(os.environ.get('CSTOP', '9'))
                for g8 in (range(4) if CS >= 5 else []):
                    for j in range(8):
                        n = g8 * 8 + j
                        b = (g8 % 2) * 2 + j % 2
                        reg = PS[:, b, (j // 2) * 128:(j // 2 + 1) * 128]
                        pr = slice((n % 2) * 64, (n % 2) * 64 + 64)
                        p.mm(reg, khatR[pr, n // 2, :], vvzR[pr, n // 2, 0, :], True, False, [X + "khat", X + "vvz"], ["pb%d" % b], inc=False)
                        p.mm(reg, khatR[pr, n // 2, :], vvzR[pr, n // 2, 1, :], False, True, [X + "khat", X + "vvz"], ["pb%d" % b], inc=(j >= 6))
                    for j in (range(8) if os.environ.get('CSUB', 'r') == 'r' else []):
                        n = g8 * 8 + j
                        b = (g8 % 2) * 2 + j % 2
                        reg = PS[:, b, (j // 2) * 128:(j // 2 + 1) * 128]
                        ut = utmp[n % 2]
                        p.tt("dve", ut, reg, bones, ALU.mult, ["pb%d" % b, "const"], [X + "ut%d" % (n % 2)])
                        p.stt("dve", ST2R[:, n + 1, :], ST2[:, n, :], dl[:, n:n + 1], ut, ALU.mult, ALU.add,
                              [X + "st%d" % n, X + "dl", X + "ut%d" % (n % 2)], [X + "st%d" % (n + 1)])
                for n in (range(NC2) if CS >= 6 else []):
                    tl = slice((n // 2) * 128, (n // 2 + 1) * 128)
                    sl = slice(n * 64, (n + 1) * 64)
                    sb_ = 2 * (n % 2)
                    for hh in range(2):
                        ps_ = slice(hh * 64, (hh + 1) * 64)
                        p.mm(PS[:, sb_ + hh, 0:64], T7R[ps_, tl], T6R[ps_, sl], True, True,
                             [X + "T7", X + "T6"], ["pb%d" % (sb_ + hh)], inc=(hh == 1))
                    sc = scmR[n % 2]
                    p.tt("dve", sc, PS[:, sb_:sb_ + 2, 0:64],
                         mask64[:, n % 2:n % 2 + 1, :].to_broadcast([128, 2, 64]), ALU.mult,
                         ["pb%d" % sb_, "pb%d" % (sb_ + 1), X + "mask"], [X + "scm%d" % (n % 2)])
                    ob = 6 + (n // 8) % 2
                    reg = PS[:, ob, (n % 8) * 64:(n % 8 + 1) * 64]
                    p.mm(reg, vvzR[:, n // 2, 0, :], sc[:, 0, :], True, False, [X + "vvz", X + "scm%d" % (n % 2)], ["pb%d" % ob], inc=False)
                    p.mm(reg, vvzR[:, n // 2, 1, :], sc[:, 1, :], False, False, [X + "vvz", X + "scm%d" % (n % 2)], ["pb%d" % ob], inc=False)
                    p.mm(reg, ST2R[:, n, :], T8R[:, sl], False, True, [X + "st%d" % n, X + "T8"], ["pb%d" % ob], inc=True)
                    if n % 8 == 7:
                        p.cp("act", T4[:, (n - 7) * 64:(n + 1) * 64], PS[:, ob, :], ["pb%d" % ob, X + "dl", X + "T5"], [X + "oc"])
                mw = p.mark()
                wg_, wgt = load_w(l, 2048 + c * 128, 128)

                def ev_g2(cb, tg, b, btok, T1=T1):
                    p.act(T1[:, tg * 512:(tg + 1) * 512], PS[:, b, :], AF.Silu, [btok, X + "T8", X + "T6"], [X + "sg"])
                proj_fm(uT, wg_, wgt, 128, ev_g2, (0, 1, 2, 3))
                p.release(mw)
                fm_rmsnorm(T4.rearrange("p (a b) -> p a b", a=1), X + "oc", 1, bonesr, False, pl[:, 20 + c:21 + c], 4 + c, mixT,
                           extra=T1.rearrange("p (a b) -> p a b", a=1), extratok=X + "sg", banks=(4, 5))
                p.release(m1)
            p.release(m0)
            if l == 0:
                dump("mixC", mixT[:, 4:6, :], ["mixT"])
                if not SKIP:
                    dump("mixT", mixT, ["mixT"])
            if stop == "C":
                break

        if 'D' not in SKIP:
            m0 = p.mark()
            X = "D_"
            selh = p.tile([4, 128])
            selg = p.tile([2, 128])
            p.dma("sp", lambda e: e.dma_start(out=selh[0:36], in_=CONST["selh"]), writes=[X + "sel"])
            p.dma("sp", lambda e: e.dma_start(out=selg[0:36], in_=CONST["selg"]), writes=[X + "sel"])
            Fdt = p.tile([T])
            Fa = p.tile([T])
            dta = p.tile([NT, 8])
            alb = p.tile([NT, 4])
            edl = p.tile([NT, 4])
            fs = p.tile([NT, 4])
            nA = p.tile([1])
            ones128 = p.tile([128])
            p.memset("pool", ones128, 1.0, [X + "ones"])
            p.memset("pool", Fa[0:32, :], 0.0, [X + "Fa"])
            mw = p.mark()
            wdt = p.tile([8, 36], BF16)
            p.dma("pool", lambda e: e.dma_start(out=wdt, in_=W_DT2[l].rearrange("(k p) c -> p k c", p=128)), writes=[X + "wdt"])
            for tg in range(4):
                sl = slice(tg * 512, (tg + 1) * 512)
                b = tg % 2
                for k in range(8):
                    p.mm(PS[0:36, b, :], wdt[:, k, :], uT[:, k, sl], k == 0, k == 7, ["uT", X + "wdt"], ["pb%d" % b], inc=(k == 7))
                p.act(Fdt[0:36, sl], PS[0:36, b, :], AF.Exp, ["pb%d" % b, "const"], [X + "Fdt"], bias=pl[0:36, 76:77])
            p.act(Fdt[0:36, :], Fdt[0:36, :], AF.Ln, [X + "Fdt"], [X + "Fdt"], bias=1.0)
            p.release(mw)
            p.act(nA, pl[:, 77:78], AF.Exp, ["const"], [X + "nA"])
            p.ts("dve", nA, nA, -1.0, None, ALU.mult, None, [X + "nA"], [X + "nA"])
            p.ts("dve", Fdt[32:36, :], Fdt[32:36, :], nA[32:36, :], None, ALU.mult, None, [X + "Fdt", X + "nA"], [X + "Fdt"])
            for n in range(NT):
                sl = slice(n * 128, (n + 1) * 128)
                p.op("dve", lambda e, sl=sl: e.tensor_tensor_scan(Fa[32:36, sl], ones128[32:36, :], Fdt[32:36, sl], 0.0, ALU.mult, ALU.add),
                     [X + "Fdt", X + "ones"], [X + "Fa"])
            for n in range(NT):
                sl = slice(n * 128, (n + 1) * 128)
                p.tr(PS[:, 0, n * 4:n * 4 + 4], Fdt[0:4, sl], ident[0:4, 0:4], [X + "Fdt", "const"], ["pb0"], inc=False)
                p.tr(PS[:, 2, n * 4:n * 4 + 4], Fa[32:36, sl], ident[32:36, 32:36], [X + "Fa", "const"], ["pb2"], inc=True)
            p.cp("dve", dta[:, :, 0:4], PS[:, 0, 0:64].rearrange("p (a b) -> p a b", b=4), ["pb0"], [X + "dta"])
            p.cp("dve", dta[:, :, 4:8], PS[:, 2, 0:64].rearrange("p (a b) -> p a b", b=4), ["pb2"], [X + "dta"])
            p.mm(PS[:, 1, 0:64], e127, dta[:, :, 4:8], True, True, [X + "dta", "const"], ["pb1"])
            p.cp("dve", alb, PS[:, 1, 0:64].rearrange("p (a b) -> p a b", b=4), ["pb1"], [X + "alb"])
            p.act(edl, alb, AF.Exp, [X + "alb"], [X + "edl"])
            p.tt("dve", fs, alb, dta[:, :, 4:8], ALU.subtract, [X + "alb", X + "dta"], [X + "fs"])
            p.act(fs, fs, AF.Exp, [X + "fs"], [X + "fs"])
            p.tt("dve", fs, fs, dta[:, :, 0:4], ALU.mult, [X + "fs", X + "dta"], [X + "fs"])
            for g in range(2):
                m1 = p.mark()
                EGR = p.tile([T], F32R); EG = EGR.bitcast(F32)
                cR = [p.tile([T], F32R) for _ in range(3)]
                cF = [a.bitcast(F32) for a in cR]
                xdzR = p.tile([NT, 2, 128], F32R)
                xddR = p.tile([NT, 128], F32R)
                BtmR = p.tile([NT, 128], F32R)
                prevR = p.tile([NT + 1, 128], F32R); prev = prevR.bitcast(F32)
                MtR = [p.tile([2, 128], F32R) for _ in range(2)]
                xp = p.tile([T + 3])
                cv = p.tile([T])
                Dt = [p.tile([2, 128]) for _ in range(2)]
                Lm = [p.tile([2, 128]) for _ in range(2)]
                ytmp = [p.tile([512]) for _ in range(2)]
                ptmp = [p.tile([128]) for _ in range(2)]
                for tg in range(4):
                    sl = slice(tg * 512, (tg + 1) * 512)
                    b = 2 + tg % 2
                    p.mm(PS[:, b, :], selg[0:36, g, :], Fa[0:36, sl], True, True, [X + "Fa", X + "sel"], ["pb%d" % b])
                    p.act(EGR[:, sl], PS[:, b, :], AF.Exp, ["pb%d" % b], [X + "EG"])
                p.ts("dve", xdzR.rearrange("p a b c -> p (a b c)"), uT[:, 0:2, :].rearrange("p a b -> p (a b)"),
                     0.0, None, ALU.mult, None, ["uT"], [X + "xdz"])
                p.ts("pool", prevR[:, 0, :], ones128, 0.0, None, ALU.mult, None, [X + "ones"], [X + "pv0"])
                p.memset("pool", xp[:, 0:3], 0.0, [X + "xp"])
                for ci, ch in enumerate([g, 2 + g, 4 + g]):
                    mw = p.mark()
                    wc, wctok = load_w(l, 2560 + ch * 128, 128)

                    def ev_c(cb, tg, b, btok):
                        evac_copy(xp[:, 3 + tg * 512:3 + (tg + 1) * 512], PS[:, b, :], [btok], [X + "xp"])
                    proj_fm(uT, wc, wctok, 128, ev_c, (4, 5, 6, 7))
                    p.release(mw)
                    cw = 44 + ch * 4
                    p.ts("dve", cv, xp[:, 3:3 + T], pl[:, cw + 3:cw + 4], pl[:, 68 + ch:69 + ch], ALU.mult, ALU.add,
                         [X + "xp", "const"], [X + "cv"])
                    for k in range(3):
                        p.stt("dve", cv, xp[:, k:k + T], pl[:, cw + k:cw + k + 1], cv, ALU.mult, ALU.add,
                              [X + "xp", X + "cv", "const"], [X + "cv"])
                    p.act(cR[ci], cv, AF.Silu, [X + "cv"], [X + "c%d" % ci])
                for n in range(NT):
                    sl = slice(n * 128, (n + 1) * 128)
                    b = n % 2
                    p.tr(PS[:, b, 0:128], cF[0][:, sl], ident, [X + "c0", "const"], ["pb%d" % b], inc=False)
                    p.tr(PS[:, b, 128:256], cF[1][:, sl], ident, [X + "c1", "const"], ["pb%d" % b], inc=True)
                    for hh in range(2):
                        h = 2 * g + hh
                        cs = slice(hh * 64, (hh + 1) * 64)
                        p.ts("dve", xdzR[:, n, hh, cs], PS[:, b, cs], dta[:, n, h:h + 1], None, ALU.mult, None,
                             ["pb%d" % b, X + "dta"], [X + "xdz"])
                        p.ts("dve", xddR[:, n, cs], PS[:, b, cs], fs[:, n, h:h + 1], None, ALU.mult, None,
                             ["pb%d" % b, X + "fs"], [X + "xdd"])
                    p.cp("act", BtmR[:, n, :], PS[:, b, 128:256], ["pb%d" % b], [X + "Btm"])
                for n in range(NT):
                    b = 4 + n // 4
                    reg = PS[:, b, (n % 4) * 128:(n % 4 + 1) * 128]
                    p.mm(reg, BtmR[:, n, :], xddR[:, n, :], True, True, [X + "Btm", X + "xdd"], ["pb%d" % b], inc=(n % 4 == 3))
                for n in range(NT):
                    b = 4 + n // 4
                    reg = PS[:, b, (n % 4) * 128:(n % 4 + 1) * 128]
                    pt = ptmp[n % 2]
                    p.tt("dve", pt.rearrange("p (a b) -> p a b", b=64), prev[:, n, :].rearrange("p (a b) -> p a b", b=64),
                         edl[:, n, 2 * g:2 * g + 2].rearrange("p (a b) -> p a b", b=1).to_broadcast([128, 2, 64]), ALU.mult,
                         [X + "pv%d" % n, X + "edl"], [X + "pt%d" % (n % 2)])
                    p.tt("dve", prevR[:, n + 1, :], pt, reg, ALU.add, [X + "pt%d" % (n % 2), "pb%d" % b], [X + "pv%d" % (n + 1)])
                mw = p.mark()
                wz, wztok = load_w(l, 2304 + g * 128, 128)

                def ev_z(cb, tg, b, btok):
                    p.act(cv[:, tg * 512:(tg + 1) * 512], PS[:, b, :], AF.Silu, [btok, X + "c2"], [X + "zt"])
                proj_fm(uT, wz, wztok, 128, ev_z, (0, 1))
                p.release(mw)
                yT = xp[:, 0:T]
                for n in range(NT):
                    sl = slice(n * 128, (n + 1) * 128)
                    lb_ = 2 + n % 2
                    for hh in range(2):
                        p.mm(PS[:, lb_, hh * 128:(hh + 1) * 128], selh[0:36, 2 * g + hh, :], Fa[0:36, sl], True, True,
                             [X + "Fa", X + "sel"], ["pb%d" % lb_], inc=False)
                    gb_ = 6 + n % 2
                    p.mm(PS[:, gb_, 0:128], cR[1][:, sl], cR[2][:, sl], True, True, [X + "c1", X + "c2"], ["pb%d" % gb_], inc=True)
                    dt_, lm_, mt_ = Dt[n % 2], Lm[n % 2], MtR[n % 2]
                    for hh in range(2):
                        h = 2 * g + hh
                        p.stt("dve", dt_[:, hh, :], PS[:, lb_, hh * 128:(hh + 1) * 128], dta[:, n, 4 + h:5 + h], negle, ALU.subtract, ALU.add,
                              ["pb%d" % lb_, X + "dta", "const"], [X + "Dt%d" % (n % 2)])
                    p.act(lm_, dt_, AF.Exp, [X + "Dt%d" % (n % 2)], [X + "Lm%d" % (n % 2)])
                    p.tt("dve", mt_, lm_, PS[:, gb_, 0:128].rearrange("p (a b) -> p a b", a=1).to_broadcast([128, 2, 128]), ALU.mult,
                         [X + "Lm%d" % (n % 2), "pb%d" % gb_], [X + "Mt%d" % (n % 2)])
                    yb = (n // 4) % 2
                    db, ob = yb, 4 + yb
                    reg_d = PS[:, db, (n % 4) * 128:(n % 4 + 1) * 128]
                    reg_o = PS[:, ob, (n % 4) * 128:(n % 4 + 1) * 128]
                    p.mm(reg_d, xdzR[:, n, 0, :], mt_[:, 0, :], True, False, [X + "xdz", X + "Mt%d" % (n % 2)], ["pb%d" % db], inc=False)
                    p.mm(reg_d, xdzR[:, n, 1, :], mt_[:, 1, :], False, True, [X + "xdz", X + "Mt%d" % (n % 2)], ["pb%d" % db], inc=False)
                    p.mm(reg_o, prevR[:, n, :], cR[2][:, sl], True, True, [X + "pv%d" % n, X + "c2"], ["pb%d" % ob], inc=True)
                    if n % 4 == 3:
                        s4 = slice((n - 3) * 128, (n + 1) * 128)
                        yt = ytmp[yb]
                        p.tt("dve", yt, PS[:, ob, :], EG[:, s4], ALU.mult, ["pb%d" % ob, "pb%d" % db, X + "EG"], [X + "yt%d" % yb])
                        p.tt("dve", yT[:, s4], yt, PS[:, db, :], ALU.add, [X + "yt%d" % yb, "pb%d" % db, X + "cv", X + "xp"], [X + "y"])
                p.stt("dve", yT, cF[0], pl[:, 74 + g:75 + g], yT, ALU.mult, ALU.add, [X + "c0", X + "y", "const"], [X + "y"])
                p.tt("dve", yT, yT, cv, ALU.mult, [X + "y", X + "zt"], [X + "y"])
                fm_rmsnorm(yT.rearrange("p (a b) -> p a b", a=1), X + "y", 1, onesr, True, pl[:, 22 + g:23 + g], 6 + g, mixT, banks=(6, 7))
                p.release(m1)
            p.release(m0)
            if l == 0:
                dump("mixD", mixT[:, 6:8, :], ["mixT"])
                if not SKIP:
                    dump("mixT", mixT, ["mixT"])
            if stop == "D":
                break
        if SKIP:
            p.memset('dve', mixT, 0.0, ['mixT'])

        m0 = p.mark()
        X = "M_"
        wo = p.tile([8, D], BF16)
        p.dma("pool", lambda e: e.dma_start(out=wo, in_=W_OUT[l].rearrange("(k p) c -> p k c", p=128)), writes=[X + "wo"])
        rw = p.tile([8, NE])
        p.dma("sp", lambda e: e.dma_start(out=rw, in_=ROUTER_W[l].rearrange("(k p) c -> p k c", p=128)), writes=[X + "rw"])
        rb = p.tile([NE])
        p.dma("sp", lambda e: e.dma_start(out=rb, in_=ROUTER_B[l:l + 1, :].partition_broadcast(128)), writes=[X + "rb"])
        iota32 = p.tile([NE])
        p.dma("sp", lambda e: e.dma_start(out=iota32, in_=CONST["iota32"]), writes=[X + "iota"])
        strib = p.tile([128], BF16)
        onesb = p.tile([128], BF16)
        p.dma("pool", lambda e: e.dma_start(out=strib, in_=CONST["stri"]), writes=[X + "cb"])
        p.dma("pool", lambda e: e.dma_start(out=onesb, in_=CONST["ones"]), writes=[X + "cb"])
        cnt = p.tile([NE])
        p.memset("dve", cnt, 0.0, [X + "cnt"])
        mP = p.mark()
        hr = [p.tile([D]) for _ in range(2)]
        xh = [p.tile([D]) for _ in range(3)]
        xnT = [p.tile([8, 128]) for _ in range(2)]
        ss = p.tile([NT]); rstd = p.tile([NT])
        lg = p.tile([NE]); mx = p.tile([8]); mxi = p.tile([8], U32); idf = p.tile([8])
        nm0 = p.tile([1]); ex = p.tile([4]); esum = p.tile([1])
        maskb = p.tile([NE], BF16)
        eq = p.tile([NE]); posk = p.tile([4]); big = p.tile([4]); slf = p.tile([4])
        for i in range(int(os.environ.get("NTP7", NT))):
            ht, xt, xT_ = hr[i % 2], xh[i % 3], xnT[i % 2]
            htok, xtok, xTtok = X + "hr%d" % (i % 2), X + "xh%d" % (i % 3), X + "xT%d" % (i % 2)
            rows = slice(i * 128, (i + 1) * 128)
            p.dma("sp", lambda e, ht=ht, rows=rows: e.dma_start(out=ht, in_=src_h[rows, :]), reads=["hbuf%d" % i], writes=[htok])
            for half in range(2):
                b = half
                for k in range(8):
                    p.mm(PS[:, b, :], mixT[:, k, rows], wo[:, k, half * 512:(half + 1) * 512], k == 0, k == 7,
                         ["mixT", X + "wo"], ["pb%d" % b], inc=(k == 7))
                p.tt("dve", ht[:, half * 512:(half + 1) * 512], ht[:, half * 512:(half + 1) * 512], PS[:, b, :], ALU.add,
                     [htok, "pb%d" % b], [htok])
            p.dma("sp", lambda e, ht=ht, rows=rows: e.dma_start(out=HBUF[rows, :], in_=ht), reads=[htok], writes=["hbuf%d" % i])
            if l == 0 and i == 0:
                pass
            p.act(xt, ht, AF.Square, [htok], [xtok, X + "ss%d" % i], accum_out=ss[:, i:i + 1])
            p.act(rstd[:, i:i + 1], ss[:, i:i + 1], AF.Sqrt, [X + "ss%d" % i], [X + "rs%d" % i], bias=EPS, scale=1.0 / D)
            p.op("dve", lambda e, i=i: e.reciprocal(rstd[:, i:i + 1], rstd[:, i:i + 1]), [X + "rs%d" % i], [X + "rs%d" % i])
            p.act(xt, ht, AF.Copy, [htok, X + "rs%d" % i], [xtok], scale=rstd[:, i:i + 1])
            transpose_tile(0, xt, xtok, xT_, xTtok, pl[:, 8:16], (2, 3))
            for k in range(8):
                p.mm(PS[:, 4, 0:NE], xT_[:, k, :], rw[:, k, :], k == 0, k == 7, [xTtok, X + "rw"], ["pb4"], inc=(k == 7))
            p.tt("dve", lg, PS[:, 4, 0:NE], rb, ALU.add, ["pb4", X + "rb"], [X + "lg"])
            p.op("dve", lambda e: e.max(mx, lg), [X + "lg"], [X + "mx"])
            p.op("dve", lambda e: e.max_index(mxi, mx, lg), [X + "lg", X + "mx"], [X + "mxi"])
            p.cp("dve", idf, mxi, [X + "mxi"], [X + "idf"])
            p.ts("dve", nm0, mx[:, 0:1], -1.0, None, ALU.mult, None, [X + "mx"], [X + "nm0"])
            p.act(ex, mx[:, 0:4], AF.Exp, [X + "mx", X + "nm0"], [X + "ex", X + "esum"], bias=nm0, accum_out=esum)
            p.op("dve", lambda e: e.reciprocal(esum, esum), [X + "esum"], [X + "esum"])
            p.ts("dve", gates_all[:, i, :], ex, esum, None, ALU.mult, None, [X + "ex", X + "esum"], [X + "gates"])
            p.ts("dve", maskb, lg, mx[:, 3:4], None, ALU.is_ge, None, [X + "lg", X + "mx"], [X + "maskb"])
            p.mm(PS[:, 5, 0:NE], strib, maskb, True, True, [X + "maskb", X + "cb"], ["pb5"])
            p.mm(PS[:, 5, NE:2 * NE], onesb, maskb, True, True, [X + "maskb", X + "cb"], ["pb5"])
            for k in range(4):
                p.ts("dve", eq, iota32, idf[:, k:k + 1], None, ALU.is_equal, None, [X + "iota", X + "idf"], [X + "eq"])
                p.tt("dve", eq, eq, PS[:, 5, 0:NE], ALU.mult, [X + "eq", "pb5"], [X + "eq"])
                p.op("dve", lambda e, k=k: e.reduce_sum(posk[:, k:k + 1], eq, AX.X), [X + "eq"], [X + "posk"])
                p.ts("dve", eq, iota32, idf[:, k:k + 1], None, ALU.is_equal, None, [X + "iota", X + "idf", X + "posk"], [X + "eq"])
                p.tt("dve", eq, eq, cnt, ALU.mult, [X + "eq", X + "cnt"], [X + "eq"])
                p.op("dve", lambda e, k=k: e.reduce_sum(big[:, k:k + 1], eq, AX.X), [X + "eq"], [X + "big"])
            p.tt("dve", posk, posk, big, ALU.add, [X + "posk", X + "big"], [X + "posk"])
            p.tt("dve", cnt, cnt, PS[:, 5, NE:2 * NE], ALU.add, [X + "cnt", "pb5", X + "big"], [X + "cnt"])
            p.ts("dve", big, posk, float(CAP), 1.0e6, ALU.is_ge, ALU.mult, [X + "posk"], [X + "big"])
            p.ts("dve", slf, idf[:, 0:4], float(CAP), None, ALU.mult, None, [X + "idf"], [X + "slf"])
            p.tt("dve", slf, slf, posk, ALU.add, [X + "slf", X + "posk"], [X + "slf"])
            p.tt("dve", slf, slf, big, ALU.add, [X + "slf", X + "big"], [X + "slf"])
            p.ts("dve", slf, slf, float(NSLOT), None, ALU.min, None, [X + "slf"], [X + "slf"])
            p.cp("dve", slots_all[:, i, :], slf, [X + "slf"], [X + "slots"])
            for k in range(4):
                p.dma("pool", lambda e, xt=xt, i=i, k=k: e.indirect_dma_start(
                    out=XS, out_offset=bass.IndirectOffsetOnAxis(ap=slots_all[:, i, k:k + 1], axis=0),
                    in_=xt, in_offset=None, oob_is_err=False),
                    reads=[xtok, X + "slots", "xsz"], writes=["xs_%d_%d" % (i, k)])
        p.release(mP)
        if l == 0:
            dump("hmid", HBUF, ["hbuf%d" % i for i in range(int(os.environ.get("NTP7", NT)))], q="sp")
            dump("slots", slots_all.bitcast(F32), [X + "slots"], q="sp")
            dump("gates", gates_all, [X + "gates"], q="sp")
        if stop == "mix":
            break
        p.release(layB)

        m8 = p.mark()
        bgu = p.tile([NE * 16])
        p.dma("sp", lambda e: e.dma_start(out=bgu, in_=BGU[l]), writes=[X + "bgu"])
        gwr = [p.tile([8, 256], BF16) for _ in range(3)]
        dwr = [p.tile([8, D], BF16) for _ in range(2)]
        xer = [p.tile([NCT, D], F32R) for _ in range(2)]
        xeTr = [p.tile([8, CAP], BF16) for _ in range(2)]
        actTr = [p.tile([8, CAP], BF16) for _ in range(2)]
        glr = [p.tile([CAP]) for _ in range(2)]
        sgr = [p.tile([CAP]) for _ in range(2)]
        lnr = [p.tile([CAP]) for _ in range(2)]
        ytr = [p.tile([D]) for _ in range(2)]
        bdr = [p.tile([D]) for _ in range(2)]
        xs_toks = ["xs_%d_%d" % (i, k) for i in range(NT) for k in range(4)]
        npiece = 0
        nyt = 0
        for e_ in range(int(os.environ.get('NESIM', NE))):
            xe, xeT, actT, dw, bdb = xer[e_ % 2], xeTr[e_ % 2], actTr[e_ % 2], dwr[e_ % 2], bdr[e_ % 2]
            xetok, xeTtok, acttok, dwtok, bdtok = X + "xe%d" % (e_ % 2), X + "xeT%d" % (e_ % 2), X + "act%d" % (e_ % 2), X + "dw%d" % (e_ % 2), X + "bd%d" % (e_ % 2)
            p.dma("pool", lambda e, xe=xe, e_=e_: e.dma_start(out=xe, in_=XS[e_ * CAP:(e_ + 1) * CAP, :].rearrange("(j p) d -> p j d", p=128)),
                  reads=xs_toks + ["xsz"], writes=[xetok])
            p.dma("pool", lambda e, dw=dw, e_=e_: e.dma_start(out=dw, in_=WD[l, e_].rearrange("(j p) d -> p j d", p=128)), writes=[dwtok])
            p.dma("sp", lambda e, bdb=bdb, e_=e_: e.dma_start(out=bdb, in_=BD[l, e_:e_ + 1, :].partition_broadcast(128)), writes=[bdtok])
            xef = xe.bitcast(F32)
            for k in range(8):
                b = 6 + k % 2
                for jt in range(NCT):
                    p.tr(PS[:, b, jt * 128:(jt + 1) * 128], xef[:, jt, k * 128:(k + 1) * 128], ident, [xetok, "const"], ["pb%d" % b], inc=(jt == NCT - 1))
                if k % 2 == 0:
                    p.act(xeT[:, k, :], PS[:, b, 0:CAP], AF.Copy, ["pb%d" % b, "const"], [xeTtok], scale=pl[:, 8 + k:9 + k])
                else:
                    p.ts("dve", xeT[:, k, :], PS[:, b, 0:CAP], pl[:, 8 + k:9 + k], None, ALU.mult, None, ["pb%d" % b, "const"], [xeTtok])
            MD = int(os.environ.get('MOEDBG', '9'))
            for j in (range(8) if MD >= 2 else []):
                gw = gwr[npiece % 3]
                gwtok = X + "gw%d" % (npiece % 3)
                npiece += 1
                p.dma("pool", lambda e, gw=gw, e_=e_, j=j: e.dma_start(out=gw, in_=GU[l, e_, j].rearrange("p (k c) -> p k c", c=256), max_dma_last_dim=1024), writes=[gwtok])
                bg, bl = 2 * (j % 2), 2 * (j % 2) + 1
                for k in range(8):
                    p.mm(PS[:, bg, 0:CAP], gw[:, k, 0:128], xeT[:, k, :], k == 0, k == 7, [gwtok, xeTtok], ["pb%d" % bg], inc=(k == 7))
                for k in range(8):
                    p.mm(PS[:, bl, 0:CAP], gw[:, k, 128:256], xeT[:, k, :], k == 0, k == 7, [gwtok, xeTtok], ["pb%d" % bl], inc=(k == 7))
                gl, sg, ln = glr[j % 2], sgr[j % 2], lnr[j % 2]
                gt_, st_, lt_ = X + "gl%d" % (j % 2), X + "sg%d" % (j % 2), X + "ln%d" % (j % 2)
                bc = e_ * 16 + j * 2
                p.ts("dve", gl, PS[:, bg, 0:CAP], bgu[:, bc:bc + 1], 7.0, ALU.add, ALU.min, ["pb%d" % bg, X + "bgu"], [gt_])
                p.act(sg, gl, AF.Sigmoid, [gt_], [st_], scale=1.702)
                p.ts("dve", ln, PS[:, bl, 0:CAP], bgu[:, bc + 1:bc + 2], 7.0, ALU.add, ALU.min, ["pb%d" % bl, X + "bgu"], [lt_])
                p.ts("dve", ln, ln, -7.0, 1.0, ALU.max, ALU.add, [lt_], [lt_])
                p.tt("dve", gl, gl, sg, ALU.mult, [gt_, st_], [gt_])
                p.tt("dve", actT[:, j, :], gl, ln, ALU.mult, [gt_, lt_], [acttok])
            for jt in (range(NCT) if MD >= 3 else []):
                yt = ytr[nyt % 2]
                yttok = X + "yt%d" % (nyt % 2)
                nyt += 1
                for half in range(2):
                    b = 4 + half
                    for j in range(8):
                        p.mm(PS[:, b, :], actT[:, j, jt * 128:(jt + 1) * 128], dw[:, j, half * 512:(half + 1) * 512], j == 0, j == 7,
                             [acttok, dwtok], ["pb%d" % b], inc=(j == 7))
                    p.tt("dve", yt[:, half * 512:(half + 1) * 512], PS[:, b, :], bdb[:, half * 512:(half + 1) * 512], ALU.add,
                         ["pb%d" % b, bdtok], [yttok])
                r0 = e_ * CAP + jt * 128
                p.dma("sp", lambda e, yt=yt, r0=r0: e.dma_start(out=YS[r0:r0 + 128, :], in_=yt), reads=[yttok, xetok], writes=["ys_%d_%d" % (e_, jt)])
        p.release(m8)
        ys_toks = ["ys_%d_%d" % (e_, jt) for e_ in range(int(os.environ.get('NESIM', NE))) for jt in range(NCT)] if int(os.environ.get('MOEDBG', '9')) >= 3 else []
        last = (l == n_layers - 1) and stop is None
        hcr = [p.tile([D]) for _ in range(2)]
        ykr = [p.tile([D]) for _ in range(4)]
        if last:
            fng = p.tile([D])
            p.dma("sp", lambda e: e.dma_start(out=fng, in_=FNG.partition_broadcast(128)), writes=[X + "fng"])
            junk = p.tile([D])
            ss2 = p.tile([NT]); rs2 = p.tile([NT])
        for i in (range(int(os.environ.get("NTP7", NT))) if int(os.environ.get('MOEDBG', '9')) >= 4 else []):
            rows = slice(i * 128, (i + 1) * 128)
            hc = hcr[i % 2]
            hctok = X + "hc%d" % (i % 2)
            p.dma("sp", lambda e, hc=hc, rows=rows: e.dma_start(out=hc, in_=HBUF[rows, :]), reads=["hbuf%d" % i], writes=[hctok])
            for k in range(4):
                yk = ykr[k]
                yktok = X + "yk%d" % k
                p.dma("pool", lambda e, yk=yk, i=i, k=k: e.indirect_dma_start(
                    out=yk, out_offset=None, in_=YS, in_offset=bass.IndirectOffsetOnAxis(ap=slots_all[:, i, k:k + 1], axis=0),
                    oob_is_err=False), reads=ys_toks + [X + "slots", "xsz"], writes=[yktok])
                p.stt("dve", hc, yk, gates_all[:, i, k:k + 1], hc, ALU.mult, ALU.add, [yktok, hctok, X + "gates"], [hctok])
            if not last:
                p.dma("sp", lambda e, hc=hc, rows=rows: e.dma_start(out=HBUF[rows, :], in_=hc), reads=[hctok], writes=["hbuf%d" % i])
            else:
                p.act(junk, hc, AF.Square, [hctok], [X + "junk", X + "s2%d" % i], accum_out=ss2[:, i:i + 1])
                p.act(rs2[:, i:i + 1], ss2[:, i:i + 1], AF.Sqrt, [X + "s2%d" % i], [X + "r2%d" % i], bias=EPS, scale=1.0 / D)
                p.op("dve", lambda e, i=i: e.reciprocal(rs2[:, i:i + 1], rs2[:, i:i + 1]), [X + "r2%d" % i], [X + "r2%d" % i])
                p.stt("dve", hc, hc, rs2[:, i:i + 1], fng, ALU.mult, ALU.mult, [hctok, X + "r2%d" % i, X + "fng"], [hctok])
                tok = "out%d" % i
                p.dma("sp", lambda e, hc=hc, rows=rows: e.dma_start(out=OUT[rows, :], in_=hc), reads=[hctok], writes=[tok])
                final_tokens.append(tok)
        if l == 0:
            dump("hout", HBUF, ["hbuf%d" % i for i in range(int(os.environ.get("NTP7", NT)))], q="sp")
        p.release(m0)
        src_h = HBUF
        if stop == "moe":
            break

        p.release(lay0)

    if not dbg:
        pass
    p.finish(final_tokens)
    return nc


def kernel(**inputs):
    inp = {k: np.asarray(v) for k, v in inputs.items()}
    w = host_prep(inp)
    nc = build()
    x = np.ascontiguousarray(inp["x"], dtype=np.float32)
    nb = x.shape[0]
    in_maps = []
    for b in range(nb):
        m = dict(w)
        m["x"] = x[b]
        in_maps.append(m)
    res = run_bass_kernel_spmd(nc, in_maps, core_ids=list(range(nb)))
    return np.stack([np.asarray(r["out"]) for r in res.results], axis=0).astype(np.float32)
```
